# Optimizing a Trainium2 kernel written in Bass

```python
import jax, jax.numpy as jnp
from jax import lax
import numpy as np

D_MODEL = 2048
BATCH = 4
SEQ = 4096
DEPTH = 2

D_MIX = D_MODEL
HEAD_DIM = 64
D_ATTN = D_MIX // 2
N_HEADS = D_ATTN // HEAD_DIM
N_KV_HEADS = N_HEADS // 8
WINDOW = 128
BLOCK = WINDOW
D_CONV = D_MIX // 4
CONV_WIDTH = 3
D_LRU = D_MIX // 4
LRU_HEAD_DIM = 64
LRU_HEADS = D_LRU // LRU_HEAD_DIM
LRU_CONV_WIDTH = 4
LRU_C = 8.0
D_KV = N_KV_HEADS * HEAD_DIM
D_IN = D_ATTN + 2 * D_KV + 3 * D_CONV + 2 * D_LRU
D_FF = 5632
N_EXPERTS = 8
TOP_K = 2
D_EXPERT = D_FF // 2
N_DENSE = (DEPTH + 1) // 2
N_MOE = DEPTH // 2
EPS = 1e-6

kernel_name = "hymba_style_hybrid_swa_conv_rglru_moe"


def rmsnorm(x, g):
    xf = x.astype(jnp.float32)
    y = xf * lax.rsqrt(jnp.mean(xf * xf, axis=-1, keepdims=True) + EPS)
    return y.astype(x.dtype) * g


def causal_depthwise_conv(u, w):
    width = w.shape[0]
    s = u.shape[1]
    up = jnp.pad(u, ((0, 0), (width - 1, 0), (0, 0)))
    y = w[0] * u
    for k in range(1, width):
        y = y + w[k] * up[:, width - 1 - k: width - 1 - k + s]
    return y


def sliding_window_attention(q, k, v, sinks):
    b, s, _ = q.shape
    nb = s // BLOCK
    g = N_HEADS // N_KV_HEADS
    f32 = jnp.float32
    qb = q.astype(f32).reshape(b, nb, BLOCK, N_KV_HEADS, g, HEAD_DIM)
    kb = k.astype(f32).reshape(b, nb, BLOCK, N_KV_HEADS, HEAD_DIM)
    vb = v.astype(f32).reshape(b, nb, BLOCK, N_KV_HEADS, HEAD_DIM)
    pad = ((0, 0), (1, 0), (0, 0), (0, 0), (0, 0))
    kw = jnp.concatenate([jnp.pad(kb[:, :-1], pad), kb], axis=2)
    vw = jnp.concatenate([jnp.pad(vb[:, :-1], pad), vb], axis=2)
    scores = jnp.einsum("bnqkgd,bnskd->bnkgqs", qb, kw) * (HEAD_DIM ** -0.5)
    q_idx = jnp.arange(BLOCK)[:, None] + BLOCK
    k_idx = jnp.arange(2 * BLOCK)[None, :]
    dist = q_idx - k_idx
    in_band = (dist >= 0) & (dist < WINDOW)
    has_keys = (jnp.arange(nb) > 0)[:, None, None] | (k_idx >= BLOCK)[None]
    valid = in_band[None] & has_keys
    scores = jnp.where(valid[None, :, None, None], scores, -jnp.inf)
    sink = sinks.astype(f32).reshape(1, 1, N_KV_HEADS, g, 1, 1)
    m = jnp.maximum(scores.max(axis=-1, keepdims=True), sink)
    p = jnp.exp(scores - m)
    denom = p.sum(axis=-1, keepdims=True) + jnp.exp(sink - m)
    out = jnp.einsum("bnkgqs,bnskd->bnqkgd", p / denom, vw)
    return out.reshape(b, s, D_ATTN).astype(q.dtype)


def _linear_recurrence_combine(left, right):
    a_l, b_l = left
    a_r, b_r = right
    return a_l * a_r, a_r * b_l + b_r


def rg_lru_branch(lx, lg, conv_w, conv_b, wa, ba, wx, bx, lam):
    b, s, _ = lx.shape
    xc = causal_depthwise_conv(lx, conv_w) + conv_b
    xh = xc.reshape(b, s, LRU_HEADS, LRU_HEAD_DIM)
    r = jax.nn.sigmoid(jnp.einsum("bshi,hij->bshj", xh, wa).reshape(b, s, D_LRU) + ba)
    i = jax.nn.sigmoid(jnp.einsum("bshi,hij->bshj", xh, wx).reshape(b, s, D_LRU) + bx)
    f32 = jnp.float32
    log_a = -LRU_C * r.astype(f32) * jax.nn.softplus(-lam.astype(f32))
    a = jnp.exp(log_a)
    u = jnp.sqrt(-jnp.expm1(2.0 * log_a)) * (i * xc).astype(f32)
    _, h = lax.associative_scan(_linear_recurrence_combine, (a, u), axis=1)
    return h.astype(lx.dtype) * jax.nn.gelu(lg)


def hybrid_mixer(h, w_in, sinks, conv_w, lru_conv_w, lru_conv_b, lru_wa, lru_ba,
                 lru_wx, lru_bx, lru_lambda, mix_norm, w_out):
    sizes = (D_ATTN, D_KV, D_KV, D_CONV, D_CONV, D_CONV, D_LRU, D_LRU)
    offsets = [int(o) for o in np.cumsum(sizes)[:-1]]
    proj = h @ w_in
    q, k, v, cb, cc, cx, lx, lg = jnp.split(proj, offsets, axis=-1)
    y_attn = sliding_window_attention(q, k, v, sinks)
    y_conv = cb * causal_depthwise_conv(cc * cx, conv_w)
    y_lru = rg_lru_branch(lx, lg, lru_conv_w, lru_conv_b, lru_wa, lru_ba,
                          lru_wx, lru_bx, lru_lambda)
    g_attn, g_conv, g_lru = jnp.split(mix_norm, [D_ATTN, D_ATTN + D_CONV])
    y = jnp.concatenate([rmsnorm(y_attn, g_attn), rmsnorm(y_conv, g_conv),
                         rmsnorm(y_lru, g_lru)], axis=-1)
    return y @ w_out


def swiglu(t, wg, wu, wd):
    return (jax.nn.silu(t @ wg) * (t @ wu)) @ wd


def moe_swiglu(h, router_w, wg, wu, wd):
    b, s, d = h.shape
    t = h.reshape(b * s, d)
    logits = (t @ router_w).astype(jnp.float32)
    top_logits, top_idx = lax.top_k(logits, TOP_K)
    top_w = jax.nn.softmax(top_logits, axis=-1)
    gates = jnp.einsum("tk,tke->te", top_w,
                       jax.nn.one_hot(top_idx, N_EXPERTS, dtype=jnp.float32)).astype(h.dtype)
    out = jnp.zeros_like(t)
    for e in range(N_EXPERTS):
        out = out + gates[:, e:e + 1] * swiglu(t, wg[e], wu[e], wd[e])
    return out.reshape(b, s, d)


def setup_inputs(seed: int = 0) -> dict:
    key = jax.random.key(seed)
    ks = jax.random.split(key, 24)
    f32 = jnp.float32

    def normal(k, shape, fan_in):
        return jax.random.normal(k, shape, f32) * (fan_in ** -0.5)

    def gain(k, shape):
        return 1.0 + 0.05 * jax.random.normal(k, shape, f32)

    a0 = jax.random.uniform(ks[11], (DEPTH, D_LRU), f32, minval=0.9, maxval=0.999)
    return {
        "x": jax.random.normal(ks[0], (BATCH, SEQ, D_MODEL), f32),
        "attn_norm": gain(ks[1], (DEPTH, D_MODEL)),
        "w_in": normal(ks[2], (DEPTH, D_MODEL, D_IN), D_MODEL),
        "attn_sinks": 0.5 * jax.random.normal(ks[3], (DEPTH, N_HEADS), f32),
        "conv_w": normal(ks[4], (DEPTH, CONV_WIDTH, D_CONV), CONV_WIDTH),
        "lru_conv_w": normal(ks[5], (DEPTH, LRU_CONV_WIDTH, D_LRU), LRU_CONV_WIDTH),
        "lru_conv_b": 0.01 * jax.random.normal(ks[6], (DEPTH, D_LRU), f32),
        "lru_wa": normal(ks[7], (DEPTH, LRU_HEADS, LRU_HEAD_DIM, LRU_HEAD_DIM), LRU_HEAD_DIM),
        "lru_ba": 0.1 * jax.random.normal(ks[8], (DEPTH, D_LRU), f32),
        "lru_wx": normal(ks[9], (DEPTH, LRU_HEADS, LRU_HEAD_DIM, LRU_HEAD_DIM), LRU_HEAD_DIM),
        "lru_bx": 0.1 * jax.random.normal(ks[10], (DEPTH, D_LRU), f32),
        "lru_lambda": jnp.log(a0) - jnp.log1p(-a0),
        "mix_norm": gain(ks[12], (DEPTH, D_MIX)),
        "w_out": normal(ks[13], (DEPTH, D_MIX, D_MODEL), D_MIX),
        "ffn_norm": gain(ks[14], (DEPTH, D_MODEL)),
        "dense_w_gate": normal(ks[15], (N_DENSE, D_MODEL, D_FF), D_MODEL),
        "dense_w_up": normal(ks[16], (N_DENSE, D_MODEL, D_FF), D_MODEL),
        "dense_w_down": normal(ks[17], (N_DENSE, D_FF, D_MODEL), D_FF),
        "router_w": normal(ks[18], (N_MOE, D_MODEL, N_EXPERTS), D_MODEL),
        "expert_w_gate": normal(ks[19], (N_MOE, N_EXPERTS, D_MODEL, D_EXPERT), D_MODEL),
        "expert_w_up": normal(ks[20], (N_MOE, N_EXPERTS, D_MODEL, D_EXPERT), D_MODEL),
        "expert_w_down": normal(ks[21], (N_MOE, N_EXPERTS, D_EXPERT, D_MODEL), D_EXPERT),
        "final_norm": gain(ks[22], (D_MODEL,)),
    }


def reference(x, attn_norm, w_in, attn_sinks, conv_w, lru_conv_w, lru_conv_b, lru_wa,
              lru_ba, lru_wx, lru_bx, lru_lambda, mix_norm, w_out, ffn_norm,
              dense_w_gate, dense_w_up, dense_w_down, router_w, expert_w_gate,
              expert_w_up, expert_w_down, final_norm):
    for layer in range(DEPTH):
        h = rmsnorm(x, attn_norm[layer])
        x = x + hybrid_mixer(h, w_in[layer], attn_sinks[layer], conv_w[layer],
                             lru_conv_w[layer], lru_conv_b[layer], lru_wa[layer],
                             lru_ba[layer], lru_wx[layer], lru_bx[layer],
                             lru_lambda[layer], mix_norm[layer], w_out[layer])
        h = rmsnorm(x, ffn_norm[layer])
        j = layer // 2
        if layer % 2 == 0:
            x = x + swiglu(h, dense_w_gate[j], dense_w_up[j], dense_w_down[j])
        else:
            x = x + moe_swiglu(h, router_w[j], expert_w_gate[j], expert_w_up[j],
                               expert_w_down[j])
    return rmsnorm(x, final_norm)
```

```python
import bisect
from contextlib import ExitStack

import numpy as np
import concourse.bass as bass
import concourse.mybir as mybir
from concourse.bass_utils import run_bass_kernel_spmd

F32 = mybir.dt.float32
BF16 = mybir.dt.bfloat16
AF = mybir.ActivationFunctionType
ALU = mybir.AluOpType

D = 2048
KC = 16
T = 512
NB = 4
D_IN_R = 4096
D_FF = 5632
D_EXP = 2816
NE = 8
EPS = 1e-6
NSLOT = 3
PK = 536

SAME_ENG_SYNC = True


class Buf:
    __slots__ = ("name", "last_w", "readers", "sem")

    def __init__(self, name):
        self.name = name
        self.last_w = None
        self.readers = []
        self.sem = None


class SemRec:
    __slots__ = ("h", "count", "hist_idx", "hist_val")

    def __init__(self, h):
        self.h = h
        self.count = 0
        self.hist_idx = []
        self.hist_val = []


class Op:
    __slots__ = ("eng", "fn", "deps", "is_dma", "sig", "sem", "val", "idx", "inc", "exact")

    def __init__(self, eng, fn, is_dma, idx):
        self.eng = eng
        self.fn = fn
        self.deps = []
        self.is_dma = is_dma
        self.sig = False
        self.sem = None
        self.val = 0
        self.idx = idx
        self.inc = 1
        self.exact = False


class Sched:
    ENGS = ("pe", "act", "dve", "pool", "sp")

    def __init__(self, nc, stack):
        self.nc = nc
        self.stack = stack
        self.ops = []
        self.q = {e: [] for e in self.ENGS}
        self.esem = {}
        for e in ("pe", "act", "dve", "pool", "sp"):
            self.esem[e] = SemRec(stack.enter_context(nc.semaphore("s_" + e)))
        self.nsem = 5

    def buf(self, name):
        return Buf(name)

    def _bufsem(self, b):
        if b.sem is None:
            b.sem = SemRec(self.stack.enter_context(self.nc.semaphore("d_" + b.name)))
            self.nsem += 1
        return b.sem

    def add(self, eng, fn, reads=(), writes=(), dma=False, semkey=None):
        op = Op(eng, fn, dma, len(self.ops))
        deps = []
        for b in reads:
            if b.last_w is not None:
                deps.append(b.last_w)
        for b in writes:
            if b.last_w is not None:
                deps.append(b.last_w)
            deps.extend(b.readers)
        seen = set()
        for d in deps:
            if id(d) in seen:
                continue
            seen.add(id(d))
            if (not d.is_dma) and d.eng == eng and (eng == "pe" or not SAME_ENG_SYNC):
                continue
            d.sig = True
            op.deps.append(d)
        for b in reads:
            b.readers.append(op)
        for b in writes:
            b.last_w = op
            b.readers = []
        if dma:
            key = semkey if semkey is not None else (writes[0] if writes else reads[0])
            op.sem = self._bufsem(key)
            op.inc = 16
            op.sig = True
        else:
            op.sem = self.esem[eng]
        self.ops.append(op)
        self.q[eng].append(op)
        return op

    def finalize(self):
        for op in self.ops:
            if op.sig:
                s = op.sem
                s.count += op.inc
                op.val = s.count
                s.hist_idx.append(op.idx)
                s.hist_val.append(op.val)

    def emit_engine(self, eng, e):
        waited = {}
        for op in self.q[eng]:
            need = {}
            for d in op.deps:
                s = d.sem
                if d.is_dma and not d.exact:
                    k = bisect.bisect_left(s.hist_idx, op.idx)
                    v = s.hist_val[k - 1]
                else:
                    v = d.val
                if need.get(id(s), (None, 0))[1] < v:
                    need[id(s)] = (s, v)
            for s, v in need.values():
                if waited.get(id(s), 0) < v:
                    e.wait_ge(s.h, v)
                    waited[id(s)] = v
            inst = op.fn(e)
            if op.sig:
                inst.then_inc(op.sem.h, op.inc)


class Prog:
    def __init__(self, NH, n_layers=2, dbg=None, final=True, moe=True):
        self.NH = NH
        self.NT = NT = 2 * NH
        self.n_layers = n_layers
        self.dbg = dbg or []
        self.final = final
        self.moe = moe
        self.wspecs = []
        self.nc = None

    def build(self):
        nc = bass.Bass("TRN2", target_bir_lowering=False)
        self.nc = nc
        with ExitStack() as stack:
            self.stack = stack
            self.S = Sched(nc, stack)
            self._alloc()
            self.dry = True
            self.wspecs = []
            self._program()
            self.wspecs_all = self.wspecs
            self.dry = False
            self.wpos = 0
            self.wissued = 0
            self.psi = self.tfi = self.tbi = self.sqi = 0
            self._program()
            self.S.finalize()
            S = self.S
            print("ops:", len(S.ops), {k: len(v) for k, v in S.q.items()}, "sems:", S.nsem, flush=True)
            with nc.Block() as block:
                @block.tensor
                def _(e):
                    S.emit_engine("pe", e)

                @block.scalar
                def _(e):
                    S.emit_engine("act", e)

                @block.vector
                def _(e):
                    S.emit_engine("dve", e)

                @block.gpsimd
                def _(e):
                    S.emit_engine("pool", e)

                @block.sync
                def _(e):
                    S.emit_engine("sp", e)
        return nc

    def sb(self, name, shape, dt):
        t = self.stack.enter_context(self.nc.sbuf_tensor(name, shape, dt))
        return t, self.S.buf(name)

    def _alloc(self):
        nc, NT = self.nc, self.NT
        L = self.n_layers

        def din(name, shape, dt=F32):
            return nc.dram_tensor(name, shape, dt, kind="ExternalInput").ap()

        self.x_in = din("xT", [NT, 128, KC, T])
        self.w_in = din("w_in_r", [2, D, D_IN_R])
        self.w_out = din("w_out", [2, D, D])
        self.wg = din("dense_wg", [D, D_FF])
        self.wu = din("dense_wu", [D, D_FF])
        self.wd = din("dense_wd", [D_FF, D])
        self.ewg = din("exp_wg", [NE, D, D_EXP])
        self.ewu = din("exp_wu", [NE, D, D_EXP])
        self.ewd = din("exp_wd", [NE, D_EXP, D])
        self.router = din("router", [128, KC, NE])
        self.pvec_d = din("pvec", [128, PV_COLS])
        self.bd_d = din("lru_bd", [2, 128, 8 * 128])
        self.sinks_d = din("sinks_row", [128, 2 * 4 * 512])
        self.masks_d = din("masks", [128, 4 * 512], BF16)
        self.flag_d = din("flag", [128, 1])
        self.ident_d = din("ident", [128, 128])
        self.sel_d = din("selmat", [8, NE * 128])
        self.out_d = nc.dram_tensor("outT", [self.NH, 128, KC, T], F32, kind="ExternalOutput").ap()
        self.xs_d = nc.dram_tensor("xs_scratch", [NT, 128, KC, T], F32).ap()
        S = self.S
        self.xs_bufs = [S.buf("xs%d" % i) for i in range(NT)]
        self.out_bufs = [S.buf("outd%d" % i) for i in range(NT)]
        self.dbg_d = {}
        for name, shape in self.dbg:
            self.dbg_d[name] = nc.dram_tensor("dbg_" + name, shape, F32, kind="ExternalOutput").ap()

        self.xT, self.xT_b = self.sb("xT_sb", [128, KC, T], F32)
        self.hT, self.hT_b = self.sb("hT", [128, KC, T], BF16)
        self.yT, self.yT_b = self.sb("yT", [128, KC, T], BF16)
        self.act, self.act_b = self.sb("act", [128, 22, T], BF16)
        self.wslots = [self.sb("wslot%d" % i, [128, KC, 512], BF16) for i in range(NSLOT)]
        self.qT, self.qT_b = self.sb("qT", [128, 8, T], BF16)
        self.gate_b, self.gate_bb = self.qT, self.qT_b
        self.KTd, self.KTd_b = self.sb("KTd", [128, 2, 128 + T], BF16)
        self.Vd, self.Vd_b = self.sb("Vd", [128, NB + 1, 256], BF16)
        self.masks, self.masks_b = self.sb("masks_sb", [128, 4 * 512], BF16)
        self.flag, self.flag_b = self.sb("flag_sb", [128, 1], F32)
        self.esink, self.esink_b = self.sb("esink", [128, 2 * 4 * 512], BF16)
        self.pvec, self.pvec_b = self.sb("pvec_sb", [128, PV_COLS], F32)
        self.bd, self.bd_b = self.sb("bd_sb", [128, 2 * 8 * 128], BF16)
        self.ones_bf, self.ones_bf_b = self.sb("ones_bf", [128, 128], BF16)
        self.ident, self.ident_b = self.sb("ident_sb", [128, 128], F32)
        self.sel, self.sel_b = self.sb("sel_sb", [8, NE * 128], F32)
        self.Rg, self.Rg_b = self.sb("Rg", [128, KC, NE], F32)
        self.c1, self.c1_b = self.sb("c1", [128, 8], F32)
        self.state, self.state_b = self.sb("lru_state", [128, 4], F32)
        self.ctail, self.ctail_b = self.sb("ctail", [128, 4, 2], F32)
        self.ltail, self.ltail_b = self.sb("ltail", [128, 4, 3], F32)
        self.rs = [self.sb("rs%d" % i, [128, T], F32) for i in range(4)]
        self.sq = [self.sb("sq%d" % i, [128, T], BF16) for i in range(4)]
        self.sqi = 0
        self.tf = [self.sb("tf%d" % i, [128, T + 4], F32) for i in range(9)]
        self.tfi = 0
        self.tb = [self.sb("tb%d" % i, [128, T], BF16) for i in range(7)]
        self.tbi = 0
        self.small, self.small_b = self.sb("small", [128, 256], F32)
        self.ps = []
        for i in range(8):
            t = self.stack.enter_context(nc.psum_tensor("ps%d" % i, [128, 512], F32))
            self.ps.append((t, S.buf("ps%d" % i)))
        self.psi = 0

    def psum(self):
        r = self.ps[self.psi % 8]
        self.psi += 1
        return r

    def tmpf(self):
        r = self.tf[self.tfi % len(self.tf)]
        self.tfi += 1
        return r

    def tmpb(self):
        r = self.tb[self.tbi % len(self.tb)]
        self.tbi += 1
        return r

    def sqt(self):
        r = self.sq[self.sqi % len(self.sq)]
        self.sqi += 1
        return r

    def op(self, eng, fn, reads=(), writes=(), **kw):
        if self.dry:
            return None
        return self.S.add(eng, fn, reads, writes, **kw)

    def wget(self, src_fn, nk, ncols):
        if self.dry:
            self.wspecs.append((src_fn, nk, ncols))
            return None, None
        while self.wissued < len(self.wspecs_all) and self.wissued <= self.wpos + NSLOT - 2:
            sfn, k, n = self.wspecs_all[self.wissued]
            t, b = self.wslots[self.wissued % NSLOT]
            src = sfn()
            dst = t[:, 0:k, 0:n]
            self.S.add("pool", lambda e, dst=dst, src=src: e.dma_start(out=dst, in_=src),
                       reads=(), writes=(b,), dma=True)
            self.wissued += 1
        r = self.wslots[self.wpos % NSLOT]
        self.wpos += 1
        return r

    def _program(self):
        if not self.dry:
            self._setup()
        for layer in range(self.n_layers):
            if not self.dry:
                self._layer_setup(layer)
            for tile in range(self.NT):
                self._tile(layer, tile)
        if not self.dry:
            self.S.add("sp", lambda e: e.nop(), reads=self.out_bufs + self.dbg_bufs, writes=())

    def _setup(self):
        S, nc = self.S, self.nc
        self.dbg_bufs = []
        self.op("sp", lambda e: e.dma_start(out=self.pvec[:, :], in_=self.pvec_d), writes=(self.pvec_b,), dma=True)
        self.op("sp", lambda e: e.dma_start(out=self.masks[:, :], in_=self.masks_d), writes=(self.masks_b,), dma=True)
        self.op("sp", lambda e: e.dma_start(out=self.flag[:, :], in_=self.flag_d), writes=(self.flag_b,), dma=True)
        self.op("sp", lambda e: e.dma_start(out=self.ident[:, :], in_=self.ident_d), writes=(self.ident_b,), dma=True)
        self.op("sp", lambda e: e.dma_start(out=self.sel[:, :], in_=self.sel_d), writes=(self.sel_b,), dma=True)
        self.op("sp", lambda e: e.dma_start(out=self.Rg[:, :, :], in_=self.router), writes=(self.Rg_b,), dma=True)
        self.op("pool", lambda e: e.dma_start(out=self.bd[:, 0:1024], in_=self.bd_d[0]), writes=(self.bd_b,), dma=True)
        self.op("pool", lambda e: e.dma_start(out=self.bd[:, 1024:2048], in_=self.bd_d[1]), writes=(self.bd_b,), dma=True)
        self.op("pool", lambda e: e.dma_start(out=self.esink[:, :], in_=self.sinks_d),
                writes=(self.esink_b,), dma=True)
        self.op("dve", lambda e: e.memset(self.ones_bf[:, :], 1.0), writes=(self.ones_bf_b,))
        self.op("act", lambda e: e.activation(out=self.esink[:, :], in_=self.esink[:, :], func=AF.Exp),
                reads=(self.esink_b,), writes=(self.esink_b,))
        lam = self.pvec[:, PV["lru_lambda"]:PV["lru_lambda"] + 8]
        self.op("act", lambda e: e.activation(out=self.c1[:, :], in_=lam, func=AF.Exp, scale=-1.0),
                reads=(self.pvec_b,), writes=(self.c1_b,))
        self.op("act", lambda e: e.activation(out=self.c1[:, :], in_=self.c1[:, :], func=AF.Ln, bias=1.0),
                reads=(self.c1_b,), writes=(self.c1_b,))
        self.op("dve", lambda e: e.tensor_scalar(out=self.c1[:, :], in0=self.c1[:, :], scalar1=-8.0, scalar2=None,
                                                 op0=ALU.mult), reads=(self.c1_b,), writes=(self.c1_b,))
        g1 = self.pvec[:, PV["ffn_norm"] + KC:PV["ffn_norm"] + 2 * KC]
        for ex in range(NE):
            self.op("dve", lambda e, ex=ex: e.tensor_tensor(out=self.Rg[:, :, ex], in0=self.Rg[:, :, ex], in1=g1, op=ALU.mult),
                    reads=(self.Rg_b, self.pvec_b), writes=(self.Rg_b,))

    def _layer_setup(self, layer):
        self.op("dve", lambda e: e.memset(self.state[:, :], 0.0), writes=(self.state_b,))
        self.op("dve", lambda e: e.memset(self.ctail[:, :, :], 0.0), writes=(self.ctail_b,))
        self.op("dve", lambda e: e.memset(self.ltail[:, :, :], 0.0), writes=(self.ltail_b,))
        self.op("dve", lambda e: e.memset(self.KTd[:, :, 0:128], 0.0), writes=(self.KTd_b,))
        self.op("dve", lambda e: e.memset(self.Vd[:, 0, :], 0.0), writes=(self.Vd_b,))

    def _norm(self, gcol, out_t, out_b, rs):
        rs_t, rs_b = rs
        pst, psb = self.psum()
        for c in range(KC):
            sq_t, sq_b = self.sqt()
            self.op("act", lambda e, c=c, sq_t=sq_t: e.activation(out=sq_t[:, :], in_=self.xT[:, c, :], func=AF.Square),
                    reads=(self.xT_b,), writes=(sq_b,))
            self.op("pe", lambda e, c=c, sq_t=sq_t: e.matmul(pst[:, :], self.ones_bf[:, :], sq_t[:, :],
                                                            start=(c == 0), stop=(c == KC - 1)),
                    reads=(sq_b, self.ones_bf_b), writes=(psb,))
        self._rstd(pst, psb, rs_t, rs_b, D)
        for c in range(KC):
            self.op("dve", lambda e, c=c: e.scalar_tensor_tensor(
                out=out_t[:, c, :], in0=self.xT[:, c, :], scalar=self.pvec[:, gcol + c:gcol + c + 1],
                in1=rs_t[:, 0:T], op0=ALU.mult, op1=ALU.mult),
                reads=(self.xT_b, self.pvec_b, rs_b), writes=(out_b,))

    def _rstd(self, pst, psb, rs_t, rs_b, n):
        self.op("act", lambda e: e.activation(out=rs_t[:, 0:T], in_=pst[:, :], func=AF.Ln, scale=1.0 / n, bias=EPS),
                reads=(psb,), writes=(rs_b,))
        self.op("act", lambda e: e.activation(out=rs_t[:, 0:T], in_=rs_t[:, 0:T], func=AF.Exp, scale=-0.5),
                reads=(rs_b,), writes=(rs_b,))

    def _proj(self, wt, wb, j, rhs_t, rhs_b, nk=KC, n=T, tok0=0):
        pst, psb = self.psum()

        def fn(e):
            inst = None
            for kc in range(nk):
                inst = e.matmul(pst[:, 0:n], wt[:, kc, j * 128:(j + 1) * 128], rhs_t[:, kc, tok0:tok0 + n],
                                start=(kc == 0), stop=(kc == nk - 1))
            return inst
        self.op("pe", fn, reads=(wb, rhs_b), writes=(psb,))
        return pst, psb

    def _tile(self, layer, tile):
        L = layer
        last_layer = (layer == self.n_layers - 1)
        src = self.x_in[tile] if layer == 0 else self.xs_d[tile]
        rd = () if layer == 0 else (self.xs_bufs[tile],)
        self.op("sp", lambda e: e.dma_start(out=self.xT[:, :, :], in_=src), reads=rd, writes=(self.xT_b,), dma=True)
        self._norm(PV["attn_norm"] + L * KC, self.hT, self.hT_b, self.rs[0])
        self._dump("hT", self.hT[:, :, :], self.hT_b, eng="pool")
        if tile == self.NH:
            for t_, b_ in ((self.state[:, :], self.state_b),
                           (self.ctail[:, :, :].rearrange("p a b -> p (a b)"), self.ctail_b),
                           (self.ltail[:, :, :].rearrange("p a b -> p (a b)"), self.ltail_b)):
                self.op("dve", lambda e, t_=t_: e.tensor_scalar(out=t_, in0=t_, scalar1=self.flag[:, 0:1], scalar2=None,
                                                                 op0=ALU.mult),
                        reads=(b_, self.flag_b), writes=(b_,))
        if last_layer and self.n_layers > 1 and tile < self.NH:
            self._mixer(L, tile, partial=True)
            return
        self._mixer(L, tile)
        self._norm(PV["ffn_norm"] + L * KC, self.hT, self.hT_b, self.rs[0])
        if L % 2 == 0 or not self.moe:
            self._ffn_dense()
        else:
            self._ffn_moe()
        if last_layer and self.final:
            self._final_norm()
        if last_layer:
            if tile < self.NH:
                return
            dst, db = self.out_d[tile - self.NH], self.out_bufs[tile]
        else:
            dst, db = self.xs_d[tile], self.xs_bufs[tile]
        self.op("sp", lambda e: e.dma_start(out=dst, in_=self.xT[:, :, :]), reads=(self.xT_b,), writes=(db,),
                dma=True, semkey=self.xT_b)

    def _dump(self, name, t, b, eng="sp"):
        if self.dry or name not in self.dbg_d:
            return
        db = self.S.buf("dbgb_%s_%d" % (name, len(self.dbg_bufs)))
        self.dbg_bufs.append(db)
        if eng == "sp":
            self.op("sp", lambda e: e.dma_start(out=self.dbg_d[name], in_=t), reads=(b,), writes=(db,), dma=True)
        else:
            for c in range(KC):
                tt, tb_ = self.tmpf()
                self.op("act", lambda e, c=c, tt=tt: e.activation(out=tt[:, 0:T], in_=t[:, c, :], func=AF.Copy),
                        reads=(b,), writes=(tb_,))
                self.op("sp", lambda e, c=c, tt=tt: e.dma_start(out=self.dbg_d[name][:, c, :], in_=tt[:, 0:T]),
                        reads=(tb_,), writes=(db,), dma=True, semkey=tb_)

    def _mixer(self, L, tile, partial=False):

        def wsrc(c0, n):
            return lambda: self.w_in[L].rearrange("(kc p) n -> p kc n", p=128)[:, :, c0:c0 + n]

        halo = (not partial) or tile == self.NH - 1
        if partial:
            if halo:
                self._kv(L, wsrc)
                self._roll_halo()
                for j in range(4):
                    wt, wb = self.wget(wsrc(1536 + 384 * j, 384), KC, 384)
                    self._conv_chunk(L, j, wt, wb)
            for s in range(2):
                wt, wb = self.wget(wsrc(3072 + 512 * s, 512), KC, 512)
                for jj in range(2):
                    self._lru_chunk(L, 2 * s + jj, jj, wt, wb)
            return
        self._kv(L, wsrc)
        self._mixer_rest(L, tile, wsrc)

    def _kv(self, L, wsrc):
        wt, wb = self.wget(wsrc(0, 512), KC, 512)
        for kv in range(2):
            pst, psb = self._proj(wt, wb, kv, self.hT, self.hT_b)
            self.op("act", lambda e, kv=kv, pst=pst: e.activation(out=self.KTd[:, kv, 128:128 + T], in_=pst[:, :],
                                                                   func=AF.Copy),
                    reads=(psb,), writes=(self.KTd_b,))
        for blk in range(NB):
            pst, psb = self.psum()

            def fn(e, blk=blk, pst=pst, wt=wt):
                inst = None
                for kc in range(KC):
                    inst = e.matmul(pst[:, 0:256], self.hT[:, kc, blk * 128:(blk + 1) * 128], wt[:, kc, 256:512],
                                    start=(kc == 0), stop=(kc == KC - 1))
                return inst
            self.op("pe", fn, reads=(wb, self.hT_b), writes=(psb,))
            self.op("dve", lambda e, blk=blk, pst=pst: e.tensor_copy(out=self.Vd[:, blk + 1, :], in_=pst[:, 0:256]),
                    reads=(psb,), writes=(self.Vd_b,))

    def _mixer_rest(self, L, tile, wsrc):
        for s in range(2):
            wt, wb = self.wget(wsrc(512 + 512 * s, 512), KC, 512)
            for j in range(4):
                pst, psb = self._proj(wt, wb, j, self.hT, self.hT_b)
                ch = s * 4 + j
                eng = "act" if j % 2 == 0 else "dve"
                if eng == "act":
                    self.op("act", lambda e, ch=ch, pst=pst: e.activation(out=self.qT[:, ch, :], in_=pst[:, :], func=AF.Copy),
                            reads=(psb,), writes=(self.qT_b,))
                else:
                    self.op("dve", lambda e, ch=ch, pst=pst: e.tensor_copy(out=self.qT[:, ch, :], in_=pst[:, :]),
                            reads=(psb,), writes=(self.qT_b,))
        self._attention(L, tile)
        for j in range(4):
            wt, wb = self.wget(wsrc(1536 + 384 * j, 384), KC, 384)
            self._conv_chunk(L, j, wt, wb)
        for s in range(2):
            wt, wb = self.wget(wsrc(3072 + 512 * s, 512), KC, 512)
            for jj in range(2):
                self._lru_chunk(L, 2 * s + jj, jj, wt, wb)
        self._dump("yraw", self.yT[:, :, :], self.yT_b, eng="pool")
        self._groupnorm_wout(L)

    def _attention(self, L, tile):
        mask_prev = self.masks[:, 0:512]
        mask_cur = self.masks[:, 512:1024]
        mask_first = self.masks[:, 1024:1536]
        mask_bnd = self.masks[:, 1536:2048]
        for blk in range(NB):
            for kv in range(2):
                for par in range(2):
                    lo, hi = par * 64, par * 64 + 64
                    g = kv * 2 + par
                    rhs_q = self.qT[lo:hi, kv * 4:kv * 4 + 4, blk * 128:(blk + 1) * 128]
                    P = []
                    for which in range(2):
                        k0 = blk * 128 + which * 128
                        pst, psb = self.psum()
                        self.op("pe", lambda e, pst=pst, k0=k0, rhs_q=rhs_q, kv=kv, lo=lo, hi=hi: e.matmul(
                            pst[:, :], self.KTd[lo:hi, kv, k0:k0 + 128], rhs_q, start=True, stop=True),
                            reads=(self.KTd_b, self.qT_b), writes=(psb,))
                        et, eb = self.tmpb()
                        self.op("act", lambda e, pst=pst, et=et: e.activation(out=et[:, :], in_=pst[:, :], func=AF.Exp,
                                                                               scale=0.125),
                                reads=(psb,), writes=(eb,))
                        if which == 0:
                            m = mask_first if (tile == 0 and blk == 0) else (
                                mask_bnd if (tile == self.NH and blk == 0) else mask_prev)
                        else:
                            m = mask_cur
                        pt, pb = self.tmpb()
                        self.op("dve", lambda e, et=et, pt=pt, m=m: e.tensor_tensor(out=pt[:, :], in0=et[:, :], in1=m,
                                                                                     op=ALU.mult),
                                reads=(eb, self.masks_b), writes=(pb,))
                        P.append((pt, pb))
                    pv_t, pv_b = self.psum()
                    dn_t, dn_b = self.psum()

                    def fpv(e, blk=blk, kv=kv, P=P, pv_t=pv_t):
                        e.matmul(pv_t[:, :], self.Vd[:, blk, kv * 128:(kv + 1) * 128], P[0][0][:, :], start=True, stop=False)
                        return e.matmul(pv_t[:, :], self.Vd[:, blk + 1, kv * 128:(kv + 1) * 128], P[1][0][:, :],
                                        start=False, stop=True)
                    self.op("pe", fpv, reads=(self.Vd_b, P[0][1], P[1][1]), writes=(pv_b,))

                    def fdn(e, P=P, dn_t=dn_t):
                        e.matmul(dn_t[:, :], self.ones_bf[:, :], P[0][0][:, :], start=True, stop=False)
                        return e.matmul(dn_t[:, :], self.ones_bf[:, :], P[1][0][:, :], start=False, stop=True)
                    self.op("pe", fdn, reads=(self.ones_bf_b, P[0][1], P[1][1]), writes=(dn_b,))
                    d_t, d_b = self.tmpf()
                    es = self.esink[:, (L * 4 + g) * 512:(L * 4 + g + 1) * 512]
                    self.op("dve", lambda e, d_t=d_t, dn_t=dn_t, es=es: e.tensor_tensor(out=d_t[:, 0:T], in0=dn_t[:, :], in1=es,
                                                                                        op=ALU.add),
                            reads=(dn_b, self.esink_b), writes=(d_b,))
                    self.op("act", lambda e, d_t=d_t: e.activation(out=d_t[:, 0:T], in_=d_t[:, 0:T], func=AF.Ln),
                            reads=(d_b,), writes=(d_b,))
                    self.op("act", lambda e, d_t=d_t: e.activation(out=d_t[:, 0:T], in_=d_t[:, 0:T], func=AF.Exp, scale=-1.0),
                            reads=(d_b,), writes=(d_b,))
                    outap = self.yT[lo:hi, kv * 4:kv * 4 + 4, blk * 128:(blk + 1) * 128]
                    self.op("dve", lambda e, pv_t=pv_t, d_t=d_t, outap=outap, lo=lo, hi=hi: e.tensor_tensor(
                        out=outap, in0=pv_t[lo:hi, :].rearrange("p (i q) -> p i q", i=4),
                        in1=d_t[lo:hi, 0:T].rearrange("p (i q) -> p i q", i=4), op=ALU.mult),
                        reads=(pv_b, d_b), writes=(self.yT_b,))
        self._roll_halo()

    def _roll_halo(self):
        self.op("act", lambda e: e.activation(out=self.KTd[:, :, 0:128], in_=self.KTd[:, :, T:T + 128], func=AF.Copy),
                reads=(self.KTd_b,), writes=(self.KTd_b,))
        self.op("dve", lambda e: e.tensor_copy(out=self.Vd[:, 0, :], in_=self.Vd[:, NB, :]),
                reads=(self.Vd_b,), writes=(self.Vd_b,))

    def _conv_chunk(self, L, j, wt, wb):
        ps_cb, b_cb = self._proj(wt, wb, 0, self.hT, self.hT_b)
        ps_cc, b_cc = self._proj(wt, wb, 1, self.hT, self.hT_b)
        ps_cx, b_cx = self._proj(wt, wb, 2, self.hT, self.hT_b)
        cc_t, cc_b = self.tmpf()
        self.op("act", lambda e: e.activation(out=cc_t[:, 0:T], in_=ps_cc[:, :], func=AF.Copy), reads=(b_cc,), writes=(cc_b,))
        xh_t, xh_b = self.tmpf()
        self.op("dve", lambda e: e.tensor_copy(out=xh_t[:, 0:2], in_=self.ctail[:, j, :]), reads=(self.ctail_b,), writes=(xh_b,))
        self.op("dve", lambda e: e.tensor_tensor(out=xh_t[:, 2:2 + T], in0=ps_cx[:, :], in1=cc_t[:, 0:T], op=ALU.mult),
                reads=(b_cx, cc_b, xh_b), writes=(xh_b,))
        self.op("dve", lambda e: e.tensor_copy(out=self.ctail[:, j, :], in_=xh_t[:, T:T + 2]), reads=(xh_b,), writes=(self.ctail_b,))
        wc = PV["conv_w"] + L * 12
        ac_t, ac_b = self.tmpf()
        self.op("dve", lambda e: e.tensor_scalar(out=ac_t[:, 0:T], in0=xh_t[:, 2:2 + T], scalar1=self.pvec[:, wc + j:wc + j + 1],
                                                 scalar2=None, op0=ALU.mult),
                reads=(xh_b, self.pvec_b), writes=(ac_b,))
        for k in (1, 2):
            self.op("dve", lambda e, k=k: e.scalar_tensor_tensor(
                out=ac_t[:, 0:T], in0=xh_t[:, 2 - k:2 - k + T], scalar=self.pvec[:, wc + 4 * k + j:wc + 4 * k + j + 1],
                in1=ac_t[:, 0:T], op0=ALU.mult, op1=ALU.add),
                reads=(xh_b, self.pvec_b, ac_b), writes=(ac_b,))
        self.op("dve", lambda e: e.tensor_tensor(out=self.yT[:, 8 + j, :], in0=ps_cb[:, :], in1=ac_t[:, 0:T], op=ALU.mult),
                reads=(b_cb, ac_b), writes=(self.yT_b,))

    def _lru_chunk(self, L, j, jj, wt, wb):
        ps_lx, b_lx = self._proj(wt, wb, 2 * jj, self.hT, self.hT_b)
        ps_lg, b_lg = self._proj(wt, wb, 2 * jj + 1, self.hT, self.hT_b)
        pv = self.pvec
        lh_t, lh_b = self.tmpf()
        self.op("dve", lambda e: e.tensor_copy(out=lh_t[:, 0:3], in_=self.ltail[:, j, :]), reads=(self.ltail_b,), writes=(lh_b,))
        self.op("act", lambda e: e.activation(out=lh_t[:, 3:3 + T], in_=ps_lx[:, :], func=AF.Copy), reads=(b_lx, lh_b), writes=(lh_b,))
        self.op("dve", lambda e: e.tensor_copy(out=self.ltail[:, j, :], in_=lh_t[:, T:T + 3]), reads=(lh_b,), writes=(self.ltail_b,))
        wc = PV["lru_conv_w"] + L * 16
        bc = PV["lru_conv_b"] + L * 4 + j
        xc_t, xc_b = self.tmpf()
        self.op("dve", lambda e: e.tensor_scalar(out=xc_t[:, 0:T], in0=lh_t[:, 3:3 + T], scalar1=pv[:, wc + j:wc + j + 1],
                                                 scalar2=pv[:, bc:bc + 1], op0=ALU.mult, op1=ALU.add),
                reads=(lh_b, self.pvec_b), writes=(xc_b,))
        for k in (1, 2, 3):
            self.op("dve", lambda e, k=k: e.scalar_tensor_tensor(
                out=xc_t[:, 0:T], in0=lh_t[:, 3 - k:3 - k + T], scalar=pv[:, wc + 4 * k + j:wc + 4 * k + j + 1],
                in1=xc_t[:, 0:T], op0=ALU.mult, op1=ALU.add),
                reads=(lh_b, self.pvec_b, xc_b), writes=(xc_b,))
        xb_t, xb_b = self.tmpb()
        self.op("act", lambda e: e.activation(out=xb_t[:, :], in_=xc_t[:, 0:T], func=AF.Copy), reads=(xc_b,), writes=(xb_b,))
        gates = []
        for gi, bname in ((0, "lru_ba"), (1, "lru_bx")):
            pst, psb = self.psum()
            col = (L * 8 + gi * 4 + j) * 128
            self.op("pe", lambda e, pst=pst, col=col: e.matmul(pst[:, :], self.bd[:, col:col + 128], xb_t[:, :],
                                                                start=True, stop=True),
                    reads=(self.bd_b, xb_b), writes=(psb,))
            g_t, g_b = self.tmpf()
            bcol = PV[bname] + L * 4 + j
            self.op("act", lambda e, pst=pst, g_t=g_t, bcol=bcol: e.activation(out=g_t[:, 0:T], in_=pst[:, :], func=AF.Sigmoid,
                                                                               bias=pv[:, bcol:bcol + 1]),
                    reads=(psb, self.pvec_b), writes=(g_b,))
            gates.append((g_t, g_b))
        (r_t, r_b), (i_t, i_b) = gates
        self.op("act", lambda e: e.activation(out=r_t[:, 0:T], in_=r_t[:, 0:T], func=AF.Exp,
                                              scale=self.c1[:, L * 4 + j:L * 4 + j + 1]),
                reads=(r_b, self.c1_b), writes=(r_b,))
        s_t, s_b = self.tmpf()
        self.op("dve", lambda e: e.tensor_tensor(out=s_t[:, 0:T], in0=r_t[:, 0:T], in1=r_t[:, 0:T], op=ALU.mult),
                reads=(r_b,), writes=(s_b,))
        self.op("act", lambda e: e.activation(out=s_t[:, 0:T], in_=s_t[:, 0:T], func=AF.Sqrt, scale=-1.0, bias=1.0),
                reads=(s_b,), writes=(s_b,))
        self.op("dve", lambda e: e.tensor_tensor(out=i_t[:, 0:T], in0=i_t[:, 0:T], in1=xc_t[:, 0:T], op=ALU.mult),
                reads=(i_b, xc_b), writes=(i_b,))
        self.op("dve", lambda e: e.tensor_tensor(out=i_t[:, 0:T], in0=i_t[:, 0:T], in1=s_t[:, 0:T], op=ALU.mult),
                reads=(i_b, s_b), writes=(i_b,))
        h_t, h_b = self.tmpf()
        self.op("dve", lambda e: e.tensor_tensor_scan(out=h_t[:, 0:T], data0=r_t[:, 0:T], data1=i_t[:, 0:T],
                                                      initial=self.state[:, j:j + 1], op0=ALU.mult, op1=ALU.add),
                reads=(r_b, i_b, self.state_b), writes=(h_b,))
        self.op("dve", lambda e: e.tensor_copy(out=self.state[:, j:j + 1], in_=h_t[:, T - 1:T]), reads=(h_b,), writes=(self.state_b,))
        lg_t, lg_b = self.tmpf()
        self.op("act", lambda e: e.activation(out=lg_t[:, 0:T], in_=ps_lg[:, :], func=AF.Copy), reads=(b_lg,), writes=(lg_b,))
        u_t, u_b = self.tmpf()
        self.op("dve", lambda e: e.tensor_tensor(out=u_t[:, 0:T], in0=lg_t[:, 0:T], in1=lg_t[:, 0:T], op=ALU.mult),
                reads=(lg_b,), writes=(u_b,))
        self.op("dve", lambda e: e.tensor_scalar(out=u_t[:, 0:T], in0=u_t[:, 0:T], scalar1=0.044715, scalar2=1.0,
                                                 op0=ALU.mult, op1=ALU.add), reads=(u_b,), writes=(u_b,))
        self.op("dve", lambda e: e.tensor_tensor(out=u_t[:, 0:T], in0=u_t[:, 0:T], in1=lg_t[:, 0:T], op=ALU.mult),
                reads=(u_b, lg_b), writes=(u_b,))
        self.op("act", lambda e: e.activation(out=u_t[:, 0:T], in_=u_t[:, 0:T], func=AF.Sigmoid, scale=1.5957691216057308),
                reads=(u_b,), writes=(u_b,))
        self.op("dve", lambda e: e.tensor_tensor(out=u_t[:, 0:T], in0=u_t[:, 0:T], in1=lg_t[:, 0:T], op=ALU.mult),
                reads=(u_b, lg_b), writes=(u_b,))
        self.op("dve", lambda e: e.tensor_tensor(out=self.yT[:, 12 + j, :], in0=h_t[:, 0:T], in1=u_t[:, 0:T], op=ALU.mult),
                reads=(h_b, u_b), writes=(self.yT_b,))

    def _groupnorm_wout(self, L):
        groups = ((0, 8), (8, 12), (12, 16))
        rsg = []
        for gi, (c0, c1) in enumerate(groups):
            pst, psb = self.psum()
            for c in range(c0, c1):
                sq_t, sq_b = self.sqt()
                self.op("act", lambda e, c=c, sq_t=sq_t: e.activation(out=sq_t[:, :], in_=self.yT[:, c, :], func=AF.Square),
                        reads=(self.yT_b,), writes=(sq_b,))
                self.op("pe", lambda e, c=c, sq_t=sq_t, pst=pst, c0=c0, c1=c1: e.matmul(
                    pst[:, :], self.ones_bf[:, :], sq_t[:, :], start=(c == c0), stop=(c == c1 - 1)),
                    reads=(sq_b, self.ones_bf_b), writes=(psb,))
            rs_t, rs_b = self.rs[1 + gi]
            self._rstd(pst, psb, rs_t, rs_b, (c1 - c0) * 128)
            rsg.append((rs_t, rs_b))
        gcol = PV["mix_norm"] + L * KC
        for gi, (c0, c1) in enumerate(groups):
            rs_t, rs_b = rsg[gi]
            for c in range(c0, c1):
                self.op("dve", lambda e, c=c, rs_t=rs_t: e.scalar_tensor_tensor(
                    out=self.yT[:, c, :], in0=self.yT[:, c, :], scalar=self.pvec[:, gcol + c:gcol + c + 1],
                    in1=rs_t[:, 0:T], op0=ALU.mult, op1=ALU.mult),
                    reads=(self.yT_b, self.pvec_b, rs_b), writes=(self.yT_b,))
        self._dump("ynorm", self.yT[:, :, :], self.yT_b, eng="pool")
        for s in range(4):
            wt, wb = self.wget(lambda s=s: self.w_out[L].rearrange("(kc p) n -> p kc n", p=128)[:, :, s * 512:(s + 1) * 512],
                               KC, 512)
            for j in range(4):
                pst, psb = self._proj(wt, wb, j, self.yT, self.yT_b)
                oc = s * 4 + j
                self.op("dve", lambda e, oc=oc, pst=pst: e.tensor_tensor(out=self.xT[:, oc, :], in0=self.xT[:, oc, :],
                                                                         in1=pst[:, :], op=ALU.add),
                        reads=(psb, self.xT_b), writes=(self.xT_b,))
        self._dump("xmid", self.xT[:, :, :], self.xT_b)

    def _swiglu_up(self, wg_src, wu_src, ncols, f0, gate=None):
        wgt, wgb = self.wget(wg_src, KC, ncols)
        wut, wub = self.wget(wu_src, KC, ncols)
        for j in range(ncols // 128):
            ps_g, b_g = self._proj(wgt, wgb, j, self.hT, self.hT_b)
            ps_u, b_u = self._proj(wut, wub, j, self.hT, self.hT_b)
            sg_t, sg_b = self.tmpb()
            self.op("act", lambda e, ps_g=ps_g, sg_t=sg_t: e.activation(out=sg_t[:, :], in_=ps_g[:, :], func=AF.Silu),
                    reads=(b_g,), writes=(sg_b,))
            f = f0 + j
            if gate is None:
                self.op("dve", lambda e, ps_u=ps_u, sg_t=sg_t, f=f: e.tensor_tensor(out=self.act[:, f, :], in0=ps_u[:, :],
                                                                                      in1=sg_t[:, :], op=ALU.mult),
                        reads=(b_u, sg_b), writes=(self.act_b,))
            else:
                tm_t, tm_b = self.tmpf()
                self.op("dve", lambda e, ps_u=ps_u, tm_t=tm_t: e.tensor_tensor(out=tm_t[:, 0:T], in0=ps_u[:, :], in1=gate,
                                                                               op=ALU.mult),
                        reads=(b_u, self.gate_bb), writes=(tm_b,))
                self.op("dve", lambda e, tm_t=tm_t, sg_t=sg_t, f=f: e.tensor_tensor(out=self.act[:, f, :], in0=tm_t[:, 0:T],
                                                                                      in1=sg_t[:, :], op=ALU.mult),
                        reads=(tm_b, sg_b), writes=(self.act_b,))

    def _down(self, wd_ap_fn, nf_total):
        fslots = []
        f = 0
        while f < nf_total:
            n = min(KC, nf_total - f)
            fslots.append((f, n))
            f += n
        for cb in range(4):
            banks = [self.psum() for _ in range(4)]
            for si, (f0, nf) in enumerate(fslots):
                wt, wb = self.wget(lambda f0=f0, nf=nf, cb=cb: wd_ap_fn().rearrange("(fc p) n -> p fc n", p=128)[
                    :, f0:f0 + nf, cb * 512:(cb + 1) * 512], nf, 512)
                for oc in range(4):
                    pst, psb = banks[oc]

                    def fn(e, pst=pst, oc=oc, f0=f0, nf=nf, si=si, wt=wt):
                        inst = None
                        for ff in range(nf):
                            inst = e.matmul(pst[:, :], wt[:, ff, oc * 128:(oc + 1) * 128], self.act[:, f0 + ff, :],
                                            start=(si == 0 and ff == 0),
                                            stop=(si == len(fslots) - 1 and ff == nf - 1))
                        return inst
                    self.op("pe", fn, reads=(wb, self.act_b), writes=(psb,))
            for oc in range(4):
                pst, psb = banks[oc]
                o = cb * 4 + oc
                self.op("dve", lambda e, o=o, pst=pst: e.tensor_tensor(out=self.xT[:, o, :], in0=self.xT[:, o, :], in1=pst[:, :],
                                                                       op=ALU.add),
                        reads=(psb, self.xT_b), writes=(self.xT_b,))

    def _ffn_dense(self):
        for half in range(2):
            c0 = half * 2816
            f = 0
            for s in range(6):
                n = 512 if s < 5 else 256
                cc = c0 + s * 512
                self._swiglu_up(lambda cc=cc, n=n: self.wg.rearrange("(kc p) n -> p kc n", p=128)[:, :, cc:cc + n],
                                lambda cc=cc, n=n: self.wu.rearrange("(kc p) n -> p kc n", p=128)[:, :, cc:cc + n],
                                n, f)
                f += n // 128
            self._down(lambda c0=c0: self.wd[c0:c0 + 2816, :], 22)

    def _ffn_moe(self):
        self._router()
        for ex in range(NE):
            f = 0
            gate = self.gate_b[:, ex, :]
            for s in range(6):
                n = 512 if s < 5 else 256
                cc = s * 512
                self._swiglu_up(lambda cc=cc, n=n, ex=ex: self.ewg[ex].rearrange("(kc p) n -> p kc n", p=128)[:, :, cc:cc + n],
                                lambda cc=cc, n=n, ex=ex: self.ewu[ex].rearrange("(kc p) n -> p kc n", p=128)[:, :, cc:cc + n],
                                n, f, gate=gate)
                f += n // 128
            self._down(lambda ex=ex: self.ewd[ex], 22)

    def _router(self):
        sm, smb = self.small, self.small_b
        rs_t, rs_b = self.rs[0]
        psl, pslb = self.psum()

        def fn(e):
            inst = None
            for kc in range(KC):
                inst = e.matmul(psl[0:NE, :], self.Rg[:, kc, :], self.xT[:, kc, :], start=(kc == 0), stop=(kc == KC - 1))
            return inst
        self.op("pe", fn, reads=(self.Rg_b, self.xT_b), writes=(pslb,))
        lt_t, lt_b = self.tmpf()
        self.op("dve", lambda e: e.tensor_tensor(out=lt_t[0:NE, 0:T], in0=psl[0:NE, :], in1=rs_t[0:NE, 0:T], op=ALU.mult),
                reads=(pslb, rs_b), writes=(lt_b,))
        ps2, ps2b = self.psum()

        def ftr(e):
            inst = None
            for blk in range(NB):
                inst = e.transpose(ps2[:, blk * NE:(blk + 1) * NE], lt_t[0:NE, blk * 128:(blk + 1) * 128],
                                   self.ident[0:NE, 0:NE])
            return inst
        self.op("pe", ftr, reads=(lt_b, self.ident_b), writes=(ps2b,))
        lg = lambda blk: sm[:, blk * NE:(blk + 1) * NE]
        self.op("act", lambda e: e.activation(out=sm[:, 0:32], in_=ps2[:, 0:32], func=AF.Copy), reads=(ps2b,), writes=(smb,))
        for blk in range(NB):
            self.op("dve", lambda e, blk=blk: e.max(out=sm[:, 32 + blk * 8:40 + blk * 8], in_=lg(blk)), reads=(smb,), writes=(smb,))
            self.op("dve", lambda e, blk=blk: e.tensor_scalar(out=sm[:, 64 + blk * 8:72 + blk * 8], in0=lg(blk),
                                                              scalar1=sm[:, 33 + blk * 8:34 + blk * 8], scalar2=None,
                                                              op0=ALU.is_ge), reads=(smb,), writes=(smb,))
            self.op("dve", lambda e, blk=blk: e.tensor_scalar(out=sm[:, 128 + blk:129 + blk], in0=sm[:, 32 + blk * 8:33 + blk * 8],
                                                              scalar1=-1.0, scalar2=None, op0=ALU.mult),
                    reads=(smb,), writes=(smb,))
            self.op("act", lambda e, blk=blk: e.activation(out=sm[:, 96 + blk * 8:104 + blk * 8], in_=lg(blk), func=AF.Exp,
                                                           bias=sm[:, 128 + blk:129 + blk]), reads=(smb,), writes=(smb,))
            self.op("dve", lambda e, blk=blk: e.tensor_tensor(out=sm[:, 96 + blk * 8:104 + blk * 8],
                                                              in0=sm[:, 96 + blk * 8:104 + blk * 8],
                                                              in1=sm[:, 64 + blk * 8:72 + blk * 8], op=ALU.mult),
                    reads=(smb,), writes=(smb,))
            self.op("dve", lambda e, blk=blk: e.reduce_sum(out=sm[:, 132 + blk:133 + blk], in_=sm[:, 96 + blk * 8:104 + blk * 8],
                                                           axis=mybir.AxisListType.X), reads=(smb,), writes=(smb,))
            self.op("dve", lambda e, blk=blk: e.reciprocal(out=sm[:, 136 + blk:137 + blk], in_=sm[:, 132 + blk:133 + blk]),
                    reads=(smb,), writes=(smb,))
            self.op("dve", lambda e, blk=blk: e.tensor_scalar(out=sm[:, 160 + blk * 8:168 + blk * 8],
                                                              in0=sm[:, 96 + blk * 8:104 + blk * 8],
                                                              scalar1=sm[:, 136 + blk:137 + blk], scalar2=None, op0=ALU.mult),
                    reads=(smb,), writes=(smb,))
        ps3, ps3b = self.psum()

        def ftr2(e):
            inst = None
            for blk in range(NB):
                inst = e.transpose(ps3[0:NE, blk * 128:(blk + 1) * 128], sm[:, 160 + blk * 8:168 + blk * 8], self.ident[:, :])
            return inst
        self.op("pe", ftr2, reads=(smb, self.ident_b), writes=(ps3b,))
        gT_t, gT_b = self.tmpf()
        self.op("act", lambda e: e.activation(out=gT_t[0:NE, 0:T], in_=ps3[0:NE, :], func=AF.Copy), reads=(ps3b,), writes=(gT_b,))
        for ex in range(NE):
            pst, psb = self.psum()
            self.op("pe", lambda e, ex=ex, pst=pst: e.matmul(pst[:, :], self.sel[0:NE, ex * 128:(ex + 1) * 128], gT_t[0:NE, 0:T],
                                                            start=True, stop=True),
                    reads=(self.sel_b, gT_b), writes=(psb,))
            self.op("act", lambda e, ex=ex, pst=pst: e.activation(out=self.gate_b[:, ex, :], in_=pst[:, :], func=AF.Copy),
                    reads=(psb,), writes=(self.gate_bb,))

    def _final_norm(self):
        rs_t, rs_b = self.rs[0]
        pst, psb = self.psum()
        for c in range(KC):
            sq_t, sq_b = self.sqt()
            self.op("act", lambda e, c=c, sq_t=sq_t: e.activation(out=sq_t[:, :], in_=self.xT[:, c, :], func=AF.Square),
                    reads=(self.xT_b,), writes=(sq_b,))
            self.op("pe", lambda e, c=c, sq_t=sq_t: e.matmul(pst[:, :], self.ones_bf[:, :], sq_t[:, :],
                                                            start=(c == 0), stop=(c == KC - 1)),
                    reads=(sq_b, self.ones_bf_b), writes=(psb,))
        self._rstd(pst, psb, rs_t, rs_b, D)
        gcol = PV["final_norm"]
        for c in range(KC):
            self.op("dve", lambda e, c=c: e.scalar_tensor_tensor(
                out=self.xT[:, c, :], in0=self.xT[:, c, :], scalar=self.pvec[:, gcol + c:gcol + c + 1],
                in1=rs_t[:, 0:T], op0=ALU.mult, op1=ALU.mult),
                reads=(self.xT_b, self.pvec_b, rs_b), writes=(self.xT_b,))


PV = {}
_c = 0
for _name, _n in (("attn_norm", 32), ("ffn_norm", 32), ("mix_norm", 32), ("final_norm", 16),
                  ("conv_w", 24), ("lru_conv_w", 32), ("lru_conv_b", 8), ("lru_ba", 8), ("lru_bx", 8),
                  ("lru_lambda", 8)):
    PV[_name] = _c
    _c += _n
PV_COLS = _c


def _fm(v):
    v = np.asarray(v, np.float32)
    lead = int(np.prod(v.shape[:-1])) if v.ndim > 1 else 1
    n = v.shape[-1] // 128
    return np.ascontiguousarray(v.reshape(lead, n, 128).transpose(2, 0, 1).reshape(128, lead * n))


def _prep_shared(inp):
    f32 = np.float32
    w_in = np.asarray(inp["w_in"], f32)
    q0, k0, v0 = 0, 1024, 1152
    cb0, cc0, cx0, lx0, lg0 = 1280, 1792, 2304, 2816, 3328
    cols = []
    for h in range(2):
        cols += [np.arange(k0 + 64 * h, k0 + 64 * h + 64)] * 2
    for h in range(2):
        cols += [np.arange(v0 + 64 * h, v0 + 64 * h + 64)] * 2
    cols.append(np.arange(q0, q0 + 1024))
    for j in range(4):
        for base in (cb0, cc0, cx0):
            cols.append(np.arange(base + 128 * j, base + 128 * j + 128))
    for j in range(4):
        for base in (lx0, lg0):
            cols.append(np.arange(base + 128 * j, base + 128 * j + 128))
    cols = np.concatenate(cols)
    assert cols.shape[0] == D_IN_R
    w_in_r = np.ascontiguousarray(w_in[:, :, cols])
    pvec = np.concatenate([
        _fm(inp["attn_norm"]), _fm(inp["ffn_norm"]), _fm(inp["mix_norm"]), _fm(inp["final_norm"]),
        _fm(inp["conv_w"]), _fm(inp["lru_conv_w"]), _fm(inp["lru_conv_b"]), _fm(inp["lru_ba"]),
        _fm(inp["lru_bx"]), _fm(inp["lru_lambda"])], axis=1)
    assert pvec.shape == (128, PV_COLS)
    bd = np.zeros((2, 128, 8 * 128), f32)
    for L in range(2):
        for gi, nm in enumerate(("lru_wa", "lru_wx")):
            w = np.asarray(inp[nm], f32)[L]
            for j in range(4):
                for hh in range(2):
                    c0 = (gi * 4 + j) * 128 + hh * 64
                    bd[L, hh * 64:(hh + 1) * 64, c0:c0 + 64] = w[2 * j + hh]
    sinks = np.asarray(inp["attn_sinks"], f32)
    srow = np.zeros((2, 4, 4, 128), f32)
    for L in range(2):
        for kv in range(2):
            for par in range(2):
                for i in range(4):
                    srow[L, kv * 2 + par, i, :] = sinks[L, kv * 8 + 2 * i + par]
    srow = np.ascontiguousarray(np.broadcast_to(srow.reshape(1, -1), (128, 4096)))
    import ml_dtypes
    kk = np.arange(128)[:, None]
    qq = np.arange(128)[None, :]
    m_prev = np.tile((kk > qq).astype(f32), (1, 4))
    m_cur = np.tile((kk <= qq).astype(f32), (1, 4))
    ident = np.eye(128, dtype=f32)
    sel = np.zeros((8, NE * 128), f32)
    for e in range(NE):
        sel[e, e * 128:(e + 1) * 128] = 1.0
    shared = {
        "w_in_r": w_in_r,
        "w_out": np.ascontiguousarray(np.asarray(inp["w_out"], f32)),
        "dense_wg": np.ascontiguousarray(np.asarray(inp["dense_w_gate"], f32)[0]),
        "dense_wu": np.ascontiguousarray(np.asarray(inp["dense_w_up"], f32)[0]),
        "dense_wd": np.ascontiguousarray(np.asarray(inp["dense_w_down"], f32)[0]),
        "exp_wg": np.ascontiguousarray(np.asarray(inp["expert_w_gate"], f32)[0]),
        "exp_wu": np.ascontiguousarray(np.asarray(inp["expert_w_up"], f32)[0]),
        "exp_wd": np.ascontiguousarray(np.asarray(inp["expert_w_down"], f32)[0]),
        "router": np.ascontiguousarray(np.asarray(inp["router_w"], f32)[0].reshape(KC, 128, NE).transpose(1, 0, 2)),
        "pvec": pvec,
        "lru_bd": bd,
        "sinks_row": srow,
        "ident": ident,
        "selmat": sel,
    }
    return shared, m_prev, m_cur


def _x_tiles(xseq, NT):
    return np.ascontiguousarray(xseq.reshape(NT, T, KC, 128).transpose(0, 3, 2, 1))


def _untile(o, NT):
    return np.ascontiguousarray(o.transpose(0, 3, 2, 1).reshape(NT * T, D))


_CACHE = {}


def run(inp, n_seq, NH, n_layers=2, dbg=None, final=True, moe=True, trace=False):
    import ml_dtypes
    shared, m_prev, m_cur = _prep_shared(inp)
    x = np.asarray(inp["x"], np.float32)
    zeros = np.zeros_like(m_prev)
    in_maps = []
    for c in range(n_seq):
        tiles = _x_tiles(x[c, :2 * NH * T], 2 * NH)
        for odd in range(2):
            m = dict(shared)
            if odd:
                m["xT"] = tiles
            else:
                m["xT"] = np.ascontiguousarray(np.concatenate([tiles[:NH], tiles[:NH]], axis=0))
            m["masks"] = np.concatenate([m_prev, m_cur, zeros, m_prev if odd else zeros], axis=1).astype(ml_dtypes.bfloat16)
            m["flag"] = np.full((128, 1), 1.0 if odd else 0.0, np.float32)
            in_maps.append(m)
    key = (NH, n_layers, str(dbg), final, moe)
    if key not in _CACHE:
        _CACHE[key] = Prog(NH, n_layers, dbg=dbg, final=final, moe=moe).build()
    nc = _CACHE[key]
    res = run_bass_kernel_spmd(nc, in_maps, core_ids=list(range(2 * n_seq)), trace=trace)
    outs = []
    for c in range(n_seq):
        outs.append(np.concatenate([_untile(np.asarray(res.results[2 * c + odd]["outT"]), NH) for odd in range(2)], axis=0))
    return outs, res


def kernel(**inputs):
    outs, _ = run(inputs, 4, 4)
    return np.stack(outs, axis=0).astype(np.float32)
```

```python
import bisect
from contextlib import ExitStack

import numpy as np
import concourse.bass as bass
import concourse.mybir as mybir
from concourse.bass_utils import run_bass_kernel_spmd

F32 = mybir.dt.float32
BF16 = mybir.dt.bfloat16
AF = mybir.ActivationFunctionType
ALU = mybir.AluOpType

D = 2048
KC = 16
T = 512
NB = 4
D_IN_R = 4096
D_FF = 5632
D_EXP = 2816
NE = 8
EPS = 1e-6
NSLOT = 3
NCV = 6
PK = 536

SAME_ENG_SYNC = True


class Buf:
    __slots__ = ("name", "last_w", "readers", "sem")

    def __init__(self, name):
        self.name = name
        self.last_w = None
        self.readers = []
        self.sem = None


class SemRec:
    __slots__ = ("h", "count", "hist_idx", "hist_val")

    def __init__(self, h):
        self.h = h
        self.count = 0
        self.hist_idx = []
        self.hist_val = []


class Op:
    __slots__ = ("eng", "fn", "deps", "is_dma", "sig", "sem", "val", "idx", "inc", "exact")

    def __init__(self, eng, fn, is_dma, idx):
        self.eng = eng
        self.fn = fn
        self.deps = []
        self.is_dma = is_dma
        self.sig = False
        self.sem = None
        self.val = 0
        self.idx = idx
        self.inc = 1
        self.exact = False


class Sched:
    ENGS = ("pe", "act", "dve", "pool", "sp")

    def __init__(self, nc, stack):
        self.nc = nc
        self.stack = stack
        self.ops = []
        self.q = {e: [] for e in self.ENGS}
        self.esem = {}
        for e in ("pe", "act", "dve", "pool", "sp"):
            self.esem[e] = SemRec(stack.enter_context(nc.semaphore("s_" + e)))
        self.nsem = 5

    def buf(self, name):
        return Buf(name)

    def _bufsem(self, b):
        if b.sem is None:
            b.sem = SemRec(self.stack.enter_context(self.nc.semaphore("d_" + b.name)))
            self.nsem += 1
        return b.sem

    def add(self, eng, fn, reads=(), writes=(), dma=False, semkey=None):
        op = Op(eng, fn, dma, len(self.ops))
        deps = []
        for b in reads:
            if b.last_w is not None:
                deps.append(b.last_w)
        for b in writes:
            if b.last_w is not None:
                deps.append(b.last_w)
            deps.extend(b.readers)
        seen = set()
        for d in deps:
            if id(d) in seen:
                continue
            seen.add(id(d))
            if (not d.is_dma) and d.eng == eng and (eng == "pe" or not SAME_ENG_SYNC):
                continue
            d.sig = True
            op.deps.append(d)
        for b in reads:
            b.readers.append(op)
        for b in writes:
            b.last_w = op
            b.readers = []
        if dma:
            key = semkey if semkey is not None else (writes[0] if writes else reads[0])
            op.sem = self._bufsem(key)
            op.inc = 16
            op.sig = True
        else:
            op.sem = self.esem[eng]
        self.ops.append(op)
        self.q[eng].append(op)
        return op

    def finalize(self):
        for op in self.ops:
            if op.sig:
                s = op.sem
                s.count += op.inc
                op.val = s.count
                s.hist_idx.append(op.idx)
                s.hist_val.append(op.val)

    def emit_engine(self, eng, e):
        waited = {}
        for op in self.q[eng]:
            need = {}
            for d in op.deps:
                s = d.sem
                if d.is_dma and not d.exact:
                    k = bisect.bisect_left(s.hist_idx, op.idx)
                    v = s.hist_val[k - 1]
                else:
                    v = d.val
                if need.get(id(s), (None, 0))[1] < v:
                    need[id(s)] = (s, v)
            for s, v in need.values():
                if waited.get(id(s), 0) < v:
                    e.wait_ge(s.h, v)
                    waited[id(s)] = v
            inst = op.fn(e)
            if op.sig:
                inst.then_inc(op.sem.h, op.inc)


class Prog:
    def __init__(self, NH, n_layers=2, dbg=None, final=True, moe=True):
        self.NH = NH
        self.NT = NT = 2 * NH
        self.n_layers = n_layers
        self.dbg = dbg or []
        self.final = final
        self.moe = moe
        self.wspecs = []
        self.nc = None

    def build(self):
        nc = bass.Bass("TRN2", target_bir_lowering=False)
        self.nc = nc
        with ExitStack() as stack:
            self.stack = stack
            self.S = Sched(nc, stack)
            self._alloc()
            self.dry = True
            self.wspecs = []
            self._program()
            self.wspecs_all = self.wspecs
            self.wuniq = {}
            self.wuniq_specs = []
            for sfn, k, n, ky in self.wspecs_all:
                if ky not in self.wuniq:
                    self.wuniq[ky] = len(self.wuniq_specs)
                    self.wuniq_specs.append((sfn, k, n))
            U = len(self.wuniq_specs)
            WPC = 64
            packs = [nc.dram_tensor("wpack%d" % i, [min(WPC, U - i * WPC), 128, KC * 512], BF16).ap()
                     for i in range((U + WPC - 1) // WPC)]
            self.wpack = [packs[u // WPC][u % WPC] for u in range(U)]
            self.pkb = [self.S.buf("pk%d" % u) for u in range(U)]
            self.cvs = [self.S.buf("cv%d" % i) for i in range(NCV)]
            print("unique weight slots:", U, flush=True)
            self.dry = False
            self.wpos = 0
            self.wissued = 0
            self.psi = self.tfi = self.tbi = self.sqi = 0
            self._program()
            self.S.finalize()
            S = self.S
            print("ops:", len(S.ops), {k: len(v) for k, v in S.q.items()}, "sems:", S.nsem, flush=True)
            with nc.Block() as block:
                @block.tensor
                def _(e):
                    S.emit_engine("pe", e)

                @block.scalar
                def _(e):
                    S.emit_engine("act", e)

                @block.vector
                def _(e):
                    S.emit_engine("dve", e)

                @block.gpsimd
                def _(e):
                    S.emit_engine("pool", e)

                @block.sync
                def _(e):
                    S.emit_engine("sp", e)
        return nc

    def sb(self, name, shape, dt):
        t = self.stack.enter_context(self.nc.sbuf_tensor(name, shape, dt))
        return t, self.S.buf(name)

    def _alloc(self):
        nc, NT = self.nc, self.NT
        L = self.n_layers

        def din(name, shape, dt=F32):
            return nc.dram_tensor(name, shape, dt, kind="ExternalInput").ap()

        self.x_in = din("xT", [NT, 128, KC, T])
        self.w_in = din("w_in_r", [2, D, D_IN_R])
        self.w_out = din("w_out", [2, D, D])
        self.wg = din("dense_wg", [D, D_FF])
        self.wu = din("dense_wu", [D, D_FF])
        self.wd = din("dense_wd", [D_FF, D])
        self.ewg = din("exp_wg", [NE, D, D_EXP])
        self.ewu = din("exp_wu", [NE, D, D_EXP])
        self.ewd = din("exp_wd", [NE, D_EXP, D])
        self.router = din("router", [128, KC, NE])
        self.pvec_d = din("pvec", [128, PV_COLS])
        self.bd_d = din("lru_bd", [2, 128, 8 * 128])
        self.sinks_d = din("sinks_row", [128, 2 * 4 * 512])
        self.masks_d = din("masks", [128, 4 * 512], BF16)
        self.flag_d = din("flag", [128, 1])
        self.ident_d = din("ident", [128, 128])
        self.sel_d = din("selmat", [8, NE * 128])
        self.out_d = nc.dram_tensor("outT", [self.NH, 128, KC, T], F32, kind="ExternalOutput").ap()
        self.xs_d = nc.dram_tensor("xs_scratch", [NT, 128, KC, T], F32).ap()
        S = self.S
        self.xs_bufs = [S.buf("xs%d" % i) for i in range(NT)]
        self.out_bufs = [S.buf("outd%d" % i) for i in range(NT)]
        self.dbg_d = {}
        for name, shape in self.dbg:
            self.dbg_d[name] = nc.dram_tensor("dbg_" + name, shape, F32, kind="ExternalOutput").ap()

        self.xT, self.xT_b = self.sb("xT_sb", [128, KC, T], F32)
        self.hT, self.hT_b = self.sb("hT", [128, KC, T], BF16)
        self.yT, self.yT_b = self.sb("yT", [128, KC, T], BF16)
        self.act, self.act_b = self.sb("act", [128, 22, T], BF16)
        self.wslots = [self.sb("wslot%d" % i, [128, KC, 512], BF16) for i in range(NSLOT)]
        self.qT, self.qT_b = self.sb("qT", [128, 8, T], BF16)
        self.gate_b, self.gate_bb = self.qT, self.qT_b
        self.KTd, self.KTd_b = self.sb("KTd", [128, 2, 128 + T], BF16)
        self.Vd, self.Vd_b = self.sb("Vd", [128, NB + 1, 256], BF16)
        self.masks, self.masks_b = self.sb("masks_sb", [128, 4 * 512], BF16)
        self.flag, self.flag_b = self.sb("flag_sb", [128, 1], F32)
        self.esink, self.esink_b = self.sb("esink", [128, 2 * 4 * 512], BF16)
        self.pvec, self.pvec_b = self.sb("pvec_sb", [128, PV_COLS], F32)
        self.bd, self.bd_b = self.sb("bd_sb", [128, 2 * 8 * 128], BF16)
        self.ones_bf, self.ones_bf_b = self.sb("ones_bf", [128, 128], BF16)
        self.ident, self.ident_b = self.sb("ident_sb", [128, 128], F32)
        self.sel, self.sel_b = self.sb("sel_sb", [8, NE * 128], F32)
        self.Rg, self.Rg_b = self.sb("Rg", [128, KC, NE], F32)
        self.c1, self.c1_b = self.sb("c1", [128, 8], F32)
        self.state, self.state_b = self.sb("lru_state", [128, 4], F32)
        self.ctail, self.ctail_b = self.sb("ctail", [128, 4, 2], F32)
        self.ltail, self.ltail_b = self.sb("ltail", [128, 4, 3], F32)
        self.rs = [self.sb("rs%d" % i, [128, T], F32) for i in range(4)]
        self.sq = [self.sb("sq%d" % i, [128, T], BF16) for i in range(4)]
        self.sqi = 0
        self.tf = [self.sb("tf%d" % i, [128, T + 4], F32) for i in range(9)]
        self.tfi = 0
        self.tb = [self.sb("tb%d" % i, [128, T], BF16) for i in range(7)]
        self.tbi = 0
        self.small, self.small_b = self.sb("small", [128, 256], F32)
        self.ps = []
        for i in range(8):
            t = self.stack.enter_context(nc.psum_tensor("ps%d" % i, [128, 512], F32))
            self.ps.append((t, S.buf("ps%d" % i)))
        self.psi = 0

    def psum(self):
        r = self.ps[self.psi % 8]
        self.psi += 1
        return r

    def tmpf(self):
        r = self.tf[self.tfi % len(self.tf)]
        self.tfi += 1
        return r

    def tmpb(self):
        r = self.tb[self.tbi % len(self.tb)]
        self.tbi += 1
        return r

    def sqt(self):
        r = self.sq[self.sqi % len(self.sq)]
        self.sqi += 1
        return r

    def op(self, eng, fn, reads=(), writes=(), **kw):
        if self.dry:
            return None
        return self.S.add(eng, fn, reads, writes, **kw)

    def wget(self, src_fn, nk, ncols, key):
        if self.dry:
            self.wspecs.append((src_fn, nk, ncols, key))
            return None, None
        while self.wissued < len(self.wspecs_all) and self.wissued <= self.wpos + NSLOT - 2:
            sfn, k, n, ky = self.wspecs_all[self.wissued]
            t, b = self.wslots[self.wissued % NSLOT]
            u = self.wuniq[ky]
            src = self.wpack[u][:, 0:k * n].rearrange("p (k n) -> p k n", k=k)
            dst = t[:, 0:k, 0:n]
            self.S.add("sp", lambda e, dst=dst, src=src: e.dma_start(out=dst, in_=src),
                       reads=(self.pkb[u],), writes=(b,), dma=True)
            self.wissued += 1
        r = self.wslots[self.wpos % NSLOT]
        self.wpos += 1
        return r

    def _pack_weights(self):
        convs = []
        for u, (sfn, k, n) in enumerate(self.wuniq_specs):
            src = sfn()
            dst = self.wpack[u][:, 0:k * n].rearrange("p (k n) -> p k n", k=k)
            op = self.S.add("pool", lambda e, dst=dst, src=src: e.dma_start(out=dst, in_=src),
                            reads=(), writes=(self.pkb[u],), dma=True, semkey=self.cvs[u % NCV])
            op.exact = True
            if u >= NCV:
                op.deps.append(convs[u - NCV])
            convs.append(op)

    def _program(self):
        if not self.dry:
            self._setup()
        for layer in range(self.n_layers):
            if not self.dry:
                self._layer_setup(layer)
            for tile in range(self.NT):
                self._tile(layer, tile)
        if not self.dry:
            self.S.add("sp", lambda e: e.nop(), reads=self.out_bufs + self.dbg_bufs, writes=())

    def _setup(self):
        S, nc = self.S, self.nc
        self.dbg_bufs = []
        self.op("sp", lambda e: e.dma_start(out=self.pvec[:, :], in_=self.pvec_d), writes=(self.pvec_b,), dma=True)
        self.op("sp", lambda e: e.dma_start(out=self.masks[:, :], in_=self.masks_d), writes=(self.masks_b,), dma=True)
        self.op("sp", lambda e: e.dma_start(out=self.flag[:, :], in_=self.flag_d), writes=(self.flag_b,), dma=True)
        self.op("sp", lambda e: e.dma_start(out=self.ident[:, :], in_=self.ident_d), writes=(self.ident_b,), dma=True)
        self.op("sp", lambda e: e.dma_start(out=self.sel[:, :], in_=self.sel_d), writes=(self.sel_b,), dma=True)
        self.op("sp", lambda e: e.dma_start(out=self.Rg[:, :, :], in_=self.router), writes=(self.Rg_b,), dma=True)
        self.op("pool", lambda e: e.dma_start(out=self.bd[:, 0:1024], in_=self.bd_d[0]), writes=(self.bd_b,), dma=True)
        self.op("pool", lambda e: e.dma_start(out=self.bd[:, 1024:2048], in_=self.bd_d[1]), writes=(self.bd_b,), dma=True)
        self.op("pool", lambda e: e.dma_start(out=self.esink[:, :], in_=self.sinks_d),
                writes=(self.esink_b,), dma=True)
        self._pack_weights()
        self.op("dve", lambda e: e.memset(self.ones_bf[:, :], 1.0), writes=(self.ones_bf_b,))
        self.op("act", lambda e: e.activation(out=self.esink[:, :], in_=self.esink[:, :], func=AF.Exp),
                reads=(self.esink_b,), writes=(self.esink_b,))
        lam = self.pvec[:, PV["lru_lambda"]:PV["lru_lambda"] + 8]
        self.op("act", lambda e: e.activation(out=self.c1[:, :], in_=lam, func=AF.Exp, scale=-1.0),
                reads=(self.pvec_b,), writes=(self.c1_b,))
        self.op("act", lambda e: e.activation(out=self.c1[:, :], in_=self.c1[:, :], func=AF.Ln, bias=1.0),
                reads=(self.c1_b,), writes=(self.c1_b,))
        self.op("dve", lambda e: e.tensor_scalar(out=self.c1[:, :], in0=self.c1[:, :], scalar1=-8.0, scalar2=None,
                                                 op0=ALU.mult), reads=(self.c1_b,), writes=(self.c1_b,))
        g1 = self.pvec[:, PV["ffn_norm"] + KC:PV["ffn_norm"] + 2 * KC]
        for ex in range(NE):
            self.op("dve", lambda e, ex=ex: e.tensor_tensor(out=self.Rg[:, :, ex], in0=self.Rg[:, :, ex], in1=g1, op=ALU.mult),
                    reads=(self.Rg_b, self.pvec_b), writes=(self.Rg_b,))

    def _layer_setup(self, layer):
        self.op("dve", lambda e: e.memset(self.state[:, :], 0.0), writes=(self.state_b,))
        self.op("dve", lambda e: e.memset(self.ctail[:, :, :], 0.0), writes=(self.ctail_b,))
        self.op("dve", lambda e: e.memset(self.ltail[:, :, :], 0.0), writes=(self.ltail_b,))
        self.op("dve", lambda e: e.memset(self.KTd[:, :, 0:128], 0.0), writes=(self.KTd_b,))
        self.op("dve", lambda e: e.memset(self.Vd[:, 0, :], 0.0), writes=(self.Vd_b,))

    def _norm(self, gcol, out_t, out_b, rs):
        rs_t, rs_b = rs
        pst, psb = self.psum()
        for c in range(KC):
            sq_t, sq_b = self.sqt()
            self.op("act", lambda e, c=c, sq_t=sq_t: e.activation(out=sq_t[:, :], in_=self.xT[:, c, :], func=AF.Square),
                    reads=(self.xT_b,), writes=(sq_b,))
            self.op("pe", lambda e, c=c, sq_t=sq_t: e.matmul(pst[:, :], self.ones_bf[:, :], sq_t[:, :],
                                                            start=(c == 0), stop=(c == KC - 1)),
                    reads=(sq_b, self.ones_bf_b), writes=(psb,))
        self._rstd(pst, psb, rs_t, rs_b, D)
        for c in range(KC):
            self.op("dve", lambda e, c=c: e.scalar_tensor_tensor(
                out=out_t[:, c, :], in0=self.xT[:, c, :], scalar=self.pvec[:, gcol + c:gcol + c + 1],
                in1=rs_t[:, 0:T], op0=ALU.mult, op1=ALU.mult),
                reads=(self.xT_b, self.pvec_b, rs_b), writes=(out_b,))

    def _rstd(self, pst, psb, rs_t, rs_b, n):
        self.op("act", lambda e: e.activation(out=rs_t[:, 0:T], in_=pst[:, :], func=AF.Ln, scale=1.0 / n, bias=EPS),
                reads=(psb,), writes=(rs_b,))
        self.op("act", lambda e: e.activation(out=rs_t[:, 0:T], in_=rs_t[:, 0:T], func=AF.Exp, scale=-0.5),
                reads=(rs_b,), writes=(rs_b,))

    def _proj(self, wt, wb, j, rhs_t, rhs_b, nk=KC, n=T, tok0=0):
        pst, psb = self.psum()

        def fn(e):
            inst = None
            for kc in range(nk):
                inst = e.matmul(pst[:, 0:n], wt[:, kc, j * 128:(j + 1) * 128], rhs_t[:, kc, tok0:tok0 + n],
                                start=(kc == 0), stop=(kc == nk - 1))
            return inst
        self.op("pe", fn, reads=(wb, rhs_b), writes=(psb,))
        return pst, psb

    def _tile(self, layer, tile):
        L = layer
        last_layer = (layer == self.n_layers - 1)
        src = self.x_in[tile] if layer == 0 else self.xs_d[tile]
        rd = () if layer == 0 else (self.xs_bufs[tile],)
        self.op("sp", lambda e: e.dma_start(out=self.xT[:, :, :], in_=src), reads=rd, writes=(self.xT_b,), dma=True)
        self._norm(PV["attn_norm"] + L * KC, self.hT, self.hT_b, self.rs[0])
        self._dump("hT", self.hT[:, :, :], self.hT_b, eng="pool")
        if tile == self.NH:
            for t_, b_ in ((self.state[:, :], self.state_b),
                           (self.ctail[:, :, :].rearrange("p a b -> p (a b)"), self.ctail_b),
                           (self.ltail[:, :, :].rearrange("p a b -> p (a b)"), self.ltail_b)):
                self.op("dve", lambda e, t_=t_: e.tensor_scalar(out=t_, in0=t_, scalar1=self.flag[:, 0:1], scalar2=None,
                                                                 op0=ALU.mult),
                        reads=(b_, self.flag_b), writes=(b_,))
        if last_layer and self.n_layers > 1 and tile < self.NH:
            self._mixer(L, tile, partial=True)
            return
        self._mixer(L, tile)
        self._norm(PV["ffn_norm"] + L * KC, self.hT, self.hT_b, self.rs[0])
        if L % 2 == 0 or not self.moe:
            self._ffn_dense()
        else:
            self._ffn_moe()
        if last_layer and self.final:
            self._final_norm()
        if last_layer:
            if tile < self.NH:
                return
            dst, db = self.out_d[tile - self.NH], self.out_bufs[tile]
        else:
            dst, db = self.xs_d[tile], self.xs_bufs[tile]
        self.op("sp", lambda e: e.dma_start(out=dst, in_=self.xT[:, :, :]), reads=(self.xT_b,), writes=(db,),
                dma=True, semkey=self.xT_b)

    def _dump(self, name, t, b, eng="sp"):
        if self.dry or name not in self.dbg_d:
            return
        db = self.S.buf("dbgb_%s_%d" % (name, len(self.dbg_bufs)))
        self.dbg_bufs.append(db)
        if eng == "sp":
            self.op("sp", lambda e: e.dma_start(out=self.dbg_d[name], in_=t), reads=(b,), writes=(db,), dma=True)
        else:
            for c in range(KC):
                tt, tb_ = self.tmpf()
                self.op("act", lambda e, c=c, tt=tt: e.activation(out=tt[:, 0:T], in_=t[:, c, :], func=AF.Copy),
                        reads=(b,), writes=(tb_,))
                self.op("sp", lambda e, c=c, tt=tt: e.dma_start(out=self.dbg_d[name][:, c, :], in_=tt[:, 0:T]),
                        reads=(tb_,), writes=(db,), dma=True, semkey=tb_)

    def _mixer(self, L, tile, partial=False):

        def wsrc(c0, n):
            return lambda: self.w_in[L].rearrange("(kc p) n -> p kc n", p=128)[:, :, c0:c0 + n]

        halo = (not partial) or tile == self.NH - 1
        if partial:
            if halo:
                self._kv(L, wsrc)
                self._roll_halo()
                for j in range(4):
                    wt, wb = self.wget(wsrc(1536 + 384 * j, 384), KC, 384, ("win", L, 1536 + 384 * j))
                    self._conv_chunk(L, j, wt, wb)
            for s in range(2):
                wt, wb = self.wget(wsrc(3072 + 512 * s, 512), KC, 512, ("win", L, 3072 + 512 * s))
                for jj in range(2):
                    self._lru_chunk(L, 2 * s + jj, jj, wt, wb)
            return
        self._kv(L, wsrc)
        self._mixer_rest(L, tile, wsrc)

    def _kv(self, L, wsrc):
        wt, wb = self.wget(wsrc(0, 512), KC, 512, ("win", L, 0))
        for kv in range(2):
            pst, psb = self._proj(wt, wb, kv, self.hT, self.hT_b)
            self.op("act", lambda e, kv=kv, pst=pst: e.activation(out=self.KTd[:, kv, 128:128 + T], in_=pst[:, :],
                                                                   func=AF.Copy),
                    reads=(psb,), writes=(self.KTd_b,))
        for blk in range(NB):
            pst, psb = self.psum()

            def fn(e, blk=blk, pst=pst, wt=wt):
                inst = None
                for kc in range(KC):
                    inst = e.matmul(pst[:, 0:256], self.hT[:, kc, blk * 128:(blk + 1) * 128], wt[:, kc, 256:512],
                                    start=(kc == 0), stop=(kc == KC - 1))
                return inst
            self.op("pe", fn, reads=(wb, self.hT_b), writes=(psb,))
            self.op("dve", lambda e, blk=blk, pst=pst: e.tensor_copy(out=self.Vd[:, blk + 1, :], in_=pst[:, 0:256]),
                    reads=(psb,), writes=(self.Vd_b,))

    def _mixer_rest(self, L, tile, wsrc):
        for s in range(2):
            wt, wb = self.wget(wsrc(512 + 512 * s, 512), KC, 512, ("win", L, 512 + 512 * s))
            for j in range(4):
                pst, psb = self._proj(wt, wb, j, self.hT, self.hT_b)
                ch = s * 4 + j
                eng = "act" if j % 2 == 0 else "dve"
                if eng == "act":
                    self.op("act", lambda e, ch=ch, pst=pst: e.activation(out=self.qT[:, ch, :], in_=pst[:, :], func=AF.Copy),
                            reads=(psb,), writes=(self.qT_b,))
                else:
                    self.op("dve", lambda e, ch=ch, pst=pst: e.tensor_copy(out=self.qT[:, ch, :], in_=pst[:, :]),
                            reads=(psb,), writes=(self.qT_b,))
        self._attention(L, tile)
        for j in range(4):
            wt, wb = self.wget(wsrc(1536 + 384 * j, 384), KC, 384, ("win", L, 1536 + 384 * j))
            self._conv_chunk(L, j, wt, wb)
        for s in range(2):
            wt, wb = self.wget(wsrc(3072 + 512 * s, 512), KC, 512, ("win", L, 3072 + 512 * s))
            for jj in range(2):
                self._lru_chunk(L, 2 * s + jj, jj, wt, wb)
        self._dump("yraw", self.yT[:, :, :], self.yT_b, eng="pool")
        self._groupnorm_wout(L)

    def _attention(self, L, tile):
        mask_prev = self.masks[:, 0:512]
        mask_cur = self.masks[:, 512:1024]
        mask_first = self.masks[:, 1024:1536]
        mask_bnd = self.masks[:, 1536:2048]
        for blk in range(NB):
            for kv in range(2):
                for par in range(2):
                    lo, hi = par * 64, par * 64 + 64
                    g = kv * 2 + par
                    rhs_q = self.qT[lo:hi, kv * 4:kv * 4 + 4, blk * 128:(blk + 1) * 128]
                    P = []
                    for which in range(2):
                        k0 = blk * 128 + which * 128
                        pst, psb = self.psum()
                        self.op("pe", lambda e, pst=pst, k0=k0, rhs_q=rhs_q, kv=kv, lo=lo, hi=hi: e.matmul(
                            pst[:, :], self.KTd[lo:hi, kv, k0:k0 + 128], rhs_q, start=True, stop=True),
                            reads=(self.KTd_b, self.qT_b), writes=(psb,))
                        et, eb = self.tmpb()
                        self.op("act", lambda e, pst=pst, et=et: e.activation(out=et[:, :], in_=pst[:, :], func=AF.Exp,
                                                                               scale=0.125),
                                reads=(psb,), writes=(eb,))
                        if which == 0:
                            m = mask_first if (tile == 0 and blk == 0) else (
                                mask_bnd if (tile == self.NH and blk == 0) else mask_prev)
                        else:
                            m = mask_cur
                        pt, pb = self.tmpb()
                        self.op("dve", lambda e, et=et, pt=pt, m=m: e.tensor_tensor(out=pt[:, :], in0=et[:, :], in1=m,
                                                                                     op=ALU.mult),
                                reads=(eb, self.masks_b), writes=(pb,))
                        P.append((pt, pb))
                    pv_t, pv_b = self.psum()
                    dn_t, dn_b = self.psum()

                    def fpv(e, blk=blk, kv=kv, P=P, pv_t=pv_t):
                        e.matmul(pv_t[:, :], self.Vd[:, blk, kv * 128:(kv + 1) * 128], P[0][0][:, :], start=True, stop=False)
                        return e.matmul(pv_t[:, :], self.Vd[:, blk + 1, kv * 128:(kv + 1) * 128], P[1][0][:, :],
                                        start=False, stop=True)
                    self.op("pe", fpv, reads=(self.Vd_b, P[0][1], P[1][1]), writes=(pv_b,))

                    def fdn(e, P=P, dn_t=dn_t):
                        e.matmul(dn_t[:, :], self.ones_bf[:, :], P[0][0][:, :], start=True, stop=False)
                        return e.matmul(dn_t[:, :], self.ones_bf[:, :], P[1][0][:, :], start=False, stop=True)
                    self.op("pe", fdn, reads=(self.ones_bf_b, P[0][1], P[1][1]), writes=(dn_b,))
                    d_t, d_b = self.tmpf()
                    es = self.esink[:, (L * 4 + g) * 512:(L * 4 + g + 1) * 512]
                    self.op("dve", lambda e, d_t=d_t, dn_t=dn_t, es=es: e.tensor_tensor(out=d_t[:, 0:T], in0=dn_t[:, :], in1=es,
                                                                                        op=ALU.add),
                            reads=(dn_b, self.esink_b), writes=(d_b,))
                    self.op("act", lambda e, d_t=d_t: e.activation(out=d_t[:, 0:T], in_=d_t[:, 0:T], func=AF.Ln),
                            reads=(d_b,), writes=(d_b,))
                    self.op("act", lambda e, d_t=d_t: e.activation(out=d_t[:, 0:T], in_=d_t[:, 0:T], func=AF.Exp, scale=-1.0),
                            reads=(d_b,), writes=(d_b,))
                    outap = self.yT[lo:hi, kv * 4:kv * 4 + 4, blk * 128:(blk + 1) * 128]
                    self.op("dve", lambda e, pv_t=pv_t, d_t=d_t, outap=outap, lo=lo, hi=hi: e.tensor_tensor(
                        out=outap, in0=pv_t[lo:hi, :].rearrange("p (i q) -> p i q", i=4),
                        in1=d_t[lo:hi, 0:T].rearrange("p (i q) -> p i q", i=4), op=ALU.mult),
                        reads=(pv_b, d_b), writes=(self.yT_b,))
        self._roll_halo()

    def _roll_halo(self):
        self.op("act", lambda e: e.activation(out=self.KTd[:, :, 0:128], in_=self.KTd[:, :, T:T + 128], func=AF.Copy),
                reads=(self.KTd_b,), writes=(self.KTd_b,))
        self.op("dve", lambda e: e.tensor_copy(out=self.Vd[:, 0, :], in_=self.Vd[:, NB, :]),
                reads=(self.Vd_b,), writes=(self.Vd_b,))

    def _conv_chunk(self, L, j, wt, wb):
        ps_cb, b_cb = self._proj(wt, wb, 0, self.hT, self.hT_b)
        ps_cc, b_cc = self._proj(wt, wb, 1, self.hT, self.hT_b)
        ps_cx, b_cx = self._proj(wt, wb, 2, self.hT, self.hT_b)
        cc_t, cc_b = self.tmpf()
        self.op("act", lambda e: e.activation(out=cc_t[:, 0:T], in_=ps_cc[:, :], func=AF.Copy), reads=(b_cc,), writes=(cc_b,))
        xh_t, xh_b = self.tmpf()
        self.op("dve", lambda e: e.tensor_copy(out=xh_t[:, 0:2], in_=self.ctail[:, j, :]), reads=(self.ctail_b,), writes=(xh_b,))
        self.op("dve", lambda e: e.tensor_tensor(out=xh_t[:, 2:2 + T], in0=ps_cx[:, :], in1=cc_t[:, 0:T], op=ALU.mult),
                reads=(b_cx, cc_b, xh_b), writes=(xh_b,))
        self.op("dve", lambda e: e.tensor_copy(out=self.ctail[:, j, :], in_=xh_t[:, T:T + 2]), reads=(xh_b,), writes=(self.ctail_b,))
        wc = PV["conv_w"] + L * 12
        ac_t, ac_b = self.tmpf()
        self.op("dve", lambda e: e.tensor_scalar(out=ac_t[:, 0:T], in0=xh_t[:, 2:2 + T], scalar1=self.pvec[:, wc + j:wc + j + 1],
                                                 scalar2=None, op0=ALU.mult),
                reads=(xh_b, self.pvec_b), writes=(ac_b,))
        for k in (1, 2):
            self.op("dve", lambda e, k=k: e.scalar_tensor_tensor(
                out=ac_t[:, 0:T], in0=xh_t[:, 2 - k:2 - k + T], scalar=self.pvec[:, wc + 4 * k + j:wc + 4 * k + j + 1],
                in1=ac_t[:, 0:T], op0=ALU.mult, op1=ALU.add),
                reads=(xh_b, self.pvec_b, ac_b), writes=(ac_b,))
        self.op("dve", lambda e: e.tensor_tensor(out=self.yT[:, 8 + j, :], in0=ps_cb[:, :], in1=ac_t[:, 0:T], op=ALU.mult),
                reads=(b_cb, ac_b), writes=(self.yT_b,))

    def _lru_chunk(self, L, j, jj, wt, wb):
        ps_lx, b_lx = self._proj(wt, wb, 2 * jj, self.hT, self.hT_b)
        ps_lg, b_lg = self._proj(wt, wb, 2 * jj + 1, self.hT, self.hT_b)
        pv = self.pvec
        lh_t, lh_b = self.tmpf()
        self.op("dve", lambda e: e.tensor_copy(out=lh_t[:, 0:3], in_=self.ltail[:, j, :]), reads=(self.ltail_b,), writes=(lh_b,))
        self.op("act", lambda e: e.activation(out=lh_t[:, 3:3 + T], in_=ps_lx[:, :], func=AF.Copy), reads=(b_lx, lh_b), writes=(lh_b,))
        self.op("dve", lambda e: e.tensor_copy(out=self.ltail[:, j, :], in_=lh_t[:, T:T + 3]), reads=(lh_b,), writes=(self.ltail_b,))
        wc = PV["lru_conv_w"] + L * 16
        bc = PV["lru_conv_b"] + L * 4 + j
        xc_t, xc_b = self.tmpf()
        self.op("dve", lambda e: e.tensor_scalar(out=xc_t[:, 0:T], in0=lh_t[:, 3:3 + T], scalar1=pv[:, wc + j:wc + j + 1],
                                                 scalar2=pv[:, bc:bc + 1], op0=ALU.mult, op1=ALU.add),
                reads=(lh_b, self.pvec_b), writes=(xc_b,))
        for k in (1, 2, 3):
            self.op("dve", lambda e, k=k: e.scalar_tensor_tensor(
                out=xc_t[:, 0:T], in0=lh_t[:, 3 - k:3 - k + T], scalar=pv[:, wc + 4 * k + j:wc + 4 * k + j + 1],
                in1=xc_t[:, 0:T], op0=ALU.mult, op1=ALU.add),
                reads=(lh_b, self.pvec_b, xc_b), writes=(xc_b,))
        xb_t, xb_b = self.tmpb()
        self.op("act", lambda e: e.activation(out=xb_t[:, :], in_=xc_t[:, 0:T], func=AF.Copy), reads=(xc_b,), writes=(xb_b,))
        gates = []
        for gi, bname in ((0, "lru_ba"), (1, "lru_bx")):
            pst, psb = self.psum()
            col = (L * 8 + gi * 4 + j) * 128
            self.op("pe", lambda e, pst=pst, col=col: e.matmul(pst[:, :], self.bd[:, col:col + 128], xb_t[:, :],
                                                                start=True, stop=True),
                    reads=(self.bd_b, xb_b), writes=(psb,))
            g_t, g_b = self.tmpf()
            bcol = PV[bname] + L * 4 + j
            self.op("act", lambda e, pst=pst, g_t=g_t, bcol=bcol: e.activation(out=g_t[:, 0:T], in_=pst[:, :], func=AF.Sigmoid,
                                                                               bias=pv[:, bcol:bcol + 1]),
                    reads=(psb, self.pvec_b), writes=(g_b,))
            gates.append((g_t, g_b))
        (r_t, r_b), (i_t, i_b) = gates
        self.op("act", lambda e: e.activation(out=r_t[:, 0:T], in_=r_t[:, 0:T], func=AF.Exp,
                                              scale=self.c1[:, L * 4 + j:L * 4 + j + 1]),
                reads=(r_b, self.c1_b), writes=(r_b,))
        s_t, s_b = self.tmpf()
        self.op("dve", lambda e: e.tensor_tensor(out=s_t[:, 0:T], in0=r_t[:, 0:T], in1=r_t[:, 0:T], op=ALU.mult),
                reads=(r_b,), writes=(s_b,))
        self.op("act", lambda e: e.activation(out=s_t[:, 0:T], in_=s_t[:, 0:T], func=AF.Sqrt, scale=-1.0, bias=1.0),
                reads=(s_b,), writes=(s_b,))
        self.op("dve", lambda e: e.tensor_tensor(out=i_t[:, 0:T], in0=i_t[:, 0:T], in1=xc_t[:, 0:T], op=ALU.mult),
                reads=(i_b, xc_b), writes=(i_b,))
        self.op("dve", lambda e: e.tensor_tensor(out=i_t[:, 0:T], in0=i_t[:, 0:T], in1=s_t[:, 0:T], op=ALU.mult),
                reads=(i_b, s_b), writes=(i_b,))
        h_t, h_b = self.tmpf()
        self.op("dve", lambda e: e.tensor_tensor_scan(out=h_t[:, 0:T], data0=r_t[:, 0:T], data1=i_t[:, 0:T],
                                                      initial=self.state[:, j:j + 1], op0=ALU.mult, op1=ALU.add),
                reads=(r_b, i_b, self.state_b), writes=(h_b,))
        self.op("dve", lambda e: e.tensor_copy(out=self.state[:, j:j + 1], in_=h_t[:, T - 1:T]), reads=(h_b,), writes=(self.state_b,))
        lg_t, lg_b = self.tmpf()
        self.op("act", lambda e: e.activation(out=lg_t[:, 0:T], in_=ps_lg[:, :], func=AF.Copy), reads=(b_lg,), writes=(lg_b,))
        u_t, u_b = self.tmpf()
        self.op("dve", lambda e: e.tensor_tensor(out=u_t[:, 0:T], in0=lg_t[:, 0:T], in1=lg_t[:, 0:T], op=ALU.mult),
                reads=(lg_b,), writes=(u_b,))
        self.op("dve", lambda e: e.tensor_scalar(out=u_t[:, 0:T], in0=u_t[:, 0:T], scalar1=0.044715, scalar2=1.0,
                                                 op0=ALU.mult, op1=ALU.add), reads=(u_b,), writes=(u_b,))
        self.op("dve", lambda e: e.tensor_tensor(out=u_t[:, 0:T], in0=u_t[:, 0:T], in1=lg_t[:, 0:T], op=ALU.mult),
                reads=(u_b, lg_b), writes=(u_b,))
        self.op("act", lambda e: e.activation(out=u_t[:, 0:T], in_=u_t[:, 0:T], func=AF.Sigmoid, scale=1.5957691216057308),
                reads=(u_b,), writes=(u_b,))
        self.op("dve", lambda e: e.tensor_tensor(out=u_t[:, 0:T], in0=u_t[:, 0:T], in1=lg_t[:, 0:T], op=ALU.mult),
                reads=(u_b, lg_b), writes=(u_b,))
        self.op("dve", lambda e: e.tensor_tensor(out=self.yT[:, 12 + j, :], in0=h_t[:, 0:T], in1=u_t[:, 0:T], op=ALU.mult),
                reads=(h_b, u_b), writes=(self.yT_b,))

    def _groupnorm_wout(self, L):
        groups = ((0, 8), (8, 12), (12, 16))
        rsg = []
        for gi, (c0, c1) in enumerate(groups):
            pst, psb = self.psum()
            for c in range(c0, c1):
                sq_t, sq_b = self.sqt()
                self.op("act", lambda e, c=c, sq_t=sq_t: e.activation(out=sq_t[:, :], in_=self.yT[:, c, :], func=AF.Square),
                        reads=(self.yT_b,), writes=(sq_b,))
                self.op("pe", lambda e, c=c, sq_t=sq_t, pst=pst, c0=c0, c1=c1: e.matmul(
                    pst[:, :], self.ones_bf[:, :], sq_t[:, :], start=(c == c0), stop=(c == c1 - 1)),
                    reads=(sq_b, self.ones_bf_b), writes=(psb,))
            rs_t, rs_b = self.rs[1 + gi]
            self._rstd(pst, psb, rs_t, rs_b, (c1 - c0) * 128)
            rsg.append((rs_t, rs_b))
        gcol = PV["mix_norm"] + L * KC
        for gi, (c0, c1) in enumerate(groups):
            rs_t, rs_b = rsg[gi]
            for c in range(c0, c1):
                self.op("dve", lambda e, c=c, rs_t=rs_t: e.scalar_tensor_tensor(
                    out=self.yT[:, c, :], in0=self.yT[:, c, :], scalar=self.pvec[:, gcol + c:gcol + c + 1],
                    in1=rs_t[:, 0:T], op0=ALU.mult, op1=ALU.mult),
                    reads=(self.yT_b, self.pvec_b, rs_b), writes=(self.yT_b,))
        self._dump("ynorm", self.yT[:, :, :], self.yT_b, eng="pool")
        for s in range(4):
            wt, wb = self.wget(lambda s=s: self.w_out[L].rearrange("(kc p) n -> p kc n", p=128)[:, :, s * 512:(s + 1) * 512],
                               KC, 512, ("wout", L, s))
            for j in range(4):
                pst, psb = self._proj(wt, wb, j, self.yT, self.yT_b)
                oc = s * 4 + j
                self.op("dve", lambda e, oc=oc, pst=pst: e.tensor_tensor(out=self.xT[:, oc, :], in0=self.xT[:, oc, :],
                                                                         in1=pst[:, :], op=ALU.add),
                        reads=(psb, self.xT_b), writes=(self.xT_b,))
        self._dump("xmid", self.xT[:, :, :], self.xT_b)

    def _swiglu_up(self, wg_src, wu_src, ncols, f0, key, gate=None):
        wgt, wgb = self.wget(wg_src, KC, ncols, ("g",) + key)
        wut, wub = self.wget(wu_src, KC, ncols, ("u",) + key)
        for j in range(ncols // 128):
            ps_g, b_g = self._proj(wgt, wgb, j, self.hT, self.hT_b)
            ps_u, b_u = self._proj(wut, wub, j, self.hT, self.hT_b)
            sg_t, sg_b = self.tmpb()
            self.op("act", lambda e, ps_g=ps_g, sg_t=sg_t: e.activation(out=sg_t[:, :], in_=ps_g[:, :], func=AF.Silu),
                    reads=(b_g,), writes=(sg_b,))
            f = f0 + j
            if gate is None:
                self.op("dve", lambda e, ps_u=ps_u, sg_t=sg_t, f=f: e.tensor_tensor(out=self.act[:, f, :], in0=ps_u[:, :],
                                                                                      in1=sg_t[:, :], op=ALU.mult),
                        reads=(b_u, sg_b), writes=(self.act_b,))
            else:
                tm_t, tm_b = self.tmpf()
                self.op("dve", lambda e, ps_u=ps_u, tm_t=tm_t: e.tensor_tensor(out=tm_t[:, 0:T], in0=ps_u[:, :], in1=gate,
                                                                               op=ALU.mult),
                        reads=(b_u, self.gate_bb), writes=(tm_b,))
                self.op("dve", lambda e, tm_t=tm_t, sg_t=sg_t, f=f: e.tensor_tensor(out=self.act[:, f, :], in0=tm_t[:, 0:T],
                                                                                      in1=sg_t[:, :], op=ALU.mult),
                        reads=(tm_b, sg_b), writes=(self.act_b,))

    def _down(self, wd_ap_fn, nf_total, key):
        fslots = []
        f = 0
        while f < nf_total:
            n = min(KC, nf_total - f)
            fslots.append((f, n))
            f += n
        for cb in range(4):
            banks = [self.psum() for _ in range(4)]
            for si, (f0, nf) in enumerate(fslots):
                wt, wb = self.wget(lambda f0=f0, nf=nf, cb=cb: wd_ap_fn().rearrange("(fc p) n -> p fc n", p=128)[
                    :, f0:f0 + nf, cb * 512:(cb + 1) * 512], nf, 512, ("d",) + key + (f0, cb))
                for oc in range(4):
                    pst, psb = banks[oc]

                    def fn(e, pst=pst, oc=oc, f0=f0, nf=nf, si=si, wt=wt):
                        inst = None
                        for ff in range(nf):
                            inst = e.matmul(pst[:, :], wt[:, ff, oc * 128:(oc + 1) * 128], self.act[:, f0 + ff, :],
                                            start=(si == 0 and ff == 0),
                                            stop=(si == len(fslots) - 1 and ff == nf - 1))
                        return inst
                    self.op("pe", fn, reads=(wb, self.act_b), writes=(psb,))
            for oc in range(4):
                pst, psb = banks[oc]
                o = cb * 4 + oc
                self.op("dve", lambda e, o=o, pst=pst: e.tensor_tensor(out=self.xT[:, o, :], in0=self.xT[:, o, :], in1=pst[:, :],
                                                                       op=ALU.add),
                        reads=(psb, self.xT_b), writes=(self.xT_b,))

    def _ffn_dense(self):
        for half in range(2):
            c0 = half * 2816
            f = 0
            for s in range(6):
                n = 512 if s < 5 else 256
                cc = c0 + s * 512
                self._swiglu_up(lambda cc=cc, n=n: self.wg.rearrange("(kc p) n -> p kc n", p=128)[:, :, cc:cc + n],
                                lambda cc=cc, n=n: self.wu.rearrange("(kc p) n -> p kc n", p=128)[:, :, cc:cc + n],
                                n, f, ("dense", cc))
                f += n // 128
            self._down(lambda c0=c0: self.wd[c0:c0 + 2816, :], 22, ("dense", c0))

    def _ffn_moe(self):
        self._router()
        for ex in range(NE):
            f = 0
            gate = self.gate_b[:, ex, :]
            for s in range(6):
                n = 512 if s < 5 else 256
                cc = s * 512
                self._swiglu_up(lambda cc=cc, n=n, ex=ex: self.ewg[ex].rearrange("(kc p) n -> p kc n", p=128)[:, :, cc:cc + n],
                                lambda cc=cc, n=n, ex=ex: self.ewu[ex].rearrange("(kc p) n -> p kc n", p=128)[:, :, cc:cc + n],
                                n, f, ("exp", ex, cc), gate=gate)
                f += n // 128
            self._down(lambda ex=ex: self.ewd[ex], 22, ("exp", ex))

    def _router(self):
        sm, smb = self.small, self.small_b
        rs_t, rs_b = self.rs[0]
        psl, pslb = self.psum()

        def fn(e):
            inst = None
            for kc in range(KC):
                inst = e.matmul(psl[0:NE, :], self.Rg[:, kc, :], self.xT[:, kc, :], start=(kc == 0), stop=(kc == KC - 1))
            return inst
        self.op("pe", fn, reads=(self.Rg_b, self.xT_b), writes=(pslb,))
        lt_t, lt_b = self.tmpf()
        self.op("dve", lambda e: e.tensor_tensor(out=lt_t[0:NE, 0:T], in0=psl[0:NE, :], in1=rs_t[0:NE, 0:T], op=ALU.mult),
                reads=(pslb, rs_b), writes=(lt_b,))
        ps2, ps2b = self.psum()

        def ftr(e):
            inst = None
            for blk in range(NB):
                inst = e.transpose(ps2[:, blk * NE:(blk + 1) * NE], lt_t[0:NE, blk * 128:(blk + 1) * 128],
                                   self.ident[0:NE, 0:NE])
            return inst
        self.op("pe", ftr, reads=(lt_b, self.ident_b), writes=(ps2b,))
        lg = lambda blk: sm[:, blk * NE:(blk + 1) * NE]
        self.op("act", lambda e: e.activation(out=sm[:, 0:32], in_=ps2[:, 0:32], func=AF.Copy), reads=(ps2b,), writes=(smb,))
        for blk in range(NB):
            self.op("dve", lambda e, blk=blk: e.max(out=sm[:, 32 + blk * 8:40 + blk * 8], in_=lg(blk)), reads=(smb,), writes=(smb,))
            self.op("dve", lambda e, blk=blk: e.tensor_scalar(out=sm[:, 64 + blk * 8:72 + blk * 8], in0=lg(blk),
                                                              scalar1=sm[:, 33 + blk * 8:34 + blk * 8], scalar2=None,
                                                              op0=ALU.is_ge), reads=(smb,), writes=(smb,))
            self.op("dve", lambda e, blk=blk: e.tensor_scalar(out=sm[:, 128 + blk:129 + blk], in0=sm[:, 32 + blk * 8:33 + blk * 8],
                                                              scalar1=-1.0, scalar2=None, op0=ALU.mult),
                    reads=(smb,), writes=(smb,))
            self.op("act", lambda e, blk=blk: e.activation(out=sm[:, 96 + blk * 8:104 + blk * 8], in_=lg(blk), func=AF.Exp,
                                                           bias=sm[:, 128 + blk:129 + blk]), reads=(smb,), writes=(smb,))
            self.op("dve", lambda e, blk=blk: e.tensor_tensor(out=sm[:, 96 + blk * 8:104 + blk * 8],
                                                              in0=sm[:, 96 + blk * 8:104 + blk * 8],
                                                              in1=sm[:, 64 + blk * 8:72 + blk * 8], op=ALU.mult),
                    reads=(smb,), writes=(smb,))
            self.op("dve", lambda e, blk=blk: e.reduce_sum(out=sm[:, 132 + blk:133 + blk], in_=sm[:, 96 + blk * 8:104 + blk * 8],
                                                           axis=mybir.AxisListType.X), reads=(smb,), writes=(smb,))
            self.op("dve", lambda e, blk=blk: e.reciprocal(out=sm[:, 136 + blk:137 + blk], in_=sm[:, 132 + blk:133 + blk]),
                    reads=(smb,), writes=(smb,))
            self.op("dve", lambda e, blk=blk: e.tensor_scalar(out=sm[:, 160 + blk * 8:168 + blk * 8],
                                                              in0=sm[:, 96 + blk * 8:104 + blk * 8],
                                                              scalar1=sm[:, 136 + blk:137 + blk], scalar2=None, op0=ALU.mult),
                    reads=(smb,), writes=(smb,))
        ps3, ps3b = self.psum()

        def ftr2(e):
            inst = None
            for blk in range(NB):
                inst = e.transpose(ps3[0:NE, blk * 128:(blk + 1) * 128], sm[:, 160 + blk * 8:168 + blk * 8], self.ident[:, :])
            return inst
        self.op("pe", ftr2, reads=(smb, self.ident_b), writes=(ps3b,))
        gT_t, gT_b = self.tmpf()
        self.op("act", lambda e: e.activation(out=gT_t[0:NE, 0:T], in_=ps3[0:NE, :], func=AF.Copy), reads=(ps3b,), writes=(gT_b,))
        for ex in range(NE):
            pst, psb = self.psum()
            self.op("pe", lambda e, ex=ex, pst=pst: e.matmul(pst[:, :], self.sel[0:NE, ex * 128:(ex + 1) * 128], gT_t[0:NE, 0:T],
                                                            start=True, stop=True),
                    reads=(self.sel_b, gT_b), writes=(psb,))
            self.op("act", lambda e, ex=ex, pst=pst: e.activation(out=self.gate_b[:, ex, :], in_=pst[:, :], func=AF.Copy),
                    reads=(psb,), writes=(self.gate_bb,))

    def _final_norm(self):
        rs_t, rs_b = self.rs[0]
        pst, psb = self.psum()
        for c in range(KC):
            sq_t, sq_b = self.sqt()
            self.op("act", lambda e, c=c, sq_t=sq_t: e.activation(out=sq_t[:, :], in_=self.xT[:, c, :], func=AF.Square),
                    reads=(self.xT_b,), writes=(sq_b,))
            self.op("pe", lambda e, c=c, sq_t=sq_t: e.matmul(pst[:, :], self.ones_bf[:, :], sq_t[:, :],
                                                            start=(c == 0), stop=(c == KC - 1)),
                    reads=(sq_b, self.ones_bf_b), writes=(psb,))
        self._rstd(pst, psb, rs_t, rs_b, D)
        gcol = PV["final_norm"]
        for c in range(KC):
            self.op("dve", lambda e, c=c: e.scalar_tensor_tensor(
                out=self.xT[:, c, :], in0=self.xT[:, c, :], scalar=self.pvec[:, gcol + c:gcol + c + 1],
                in1=rs_t[:, 0:T], op0=ALU.mult, op1=ALU.mult),
                reads=(self.xT_b, self.pvec_b, rs_b), writes=(self.xT_b,))


PV = {}
_c = 0
for _name, _n in (("attn_norm", 32), ("ffn_norm", 32), ("mix_norm", 32), ("final_norm", 16),
                  ("conv_w", 24), ("lru_conv_w", 32), ("lru_conv_b", 8), ("lru_ba", 8), ("lru_bx", 8),
                  ("lru_lambda", 8)):
    PV[_name] = _c
    _c += _n
PV_COLS = _c


def _fm(v):
    v = np.asarray(v, np.float32)
    lead = int(np.prod(v.shape[:-1])) if v.ndim > 1 else 1
    n = v.shape[-1] // 128
    return np.ascontiguousarray(v.reshape(lead, n, 128).transpose(2, 0, 1).reshape(128, lead * n))


def _prep_shared(inp):
    f32 = np.float32
    w_in = np.asarray(inp["w_in"], f32)
    q0, k0, v0 = 0, 1024, 1152
    cb0, cc0, cx0, lx0, lg0 = 1280, 1792, 2304, 2816, 3328
    cols = []
    for h in range(2):
        cols += [np.arange(k0 + 64 * h, k0 + 64 * h + 64)] * 2
    for h in range(2):
        cols += [np.arange(v0 + 64 * h, v0 + 64 * h + 64)] * 2
    cols.append(np.arange(q0, q0 + 1024))
    for j in range(4):
        for base in (cb0, cc0, cx0):
            cols.append(np.arange(base + 128 * j, base + 128 * j + 128))
    for j in range(4):
        for base in (lx0, lg0):
            cols.append(np.arange(base + 128 * j, base + 128 * j + 128))
    cols = np.concatenate(cols)
    assert cols.shape[0] == D_IN_R
    w_in_r = np.ascontiguousarray(w_in[:, :, cols])
    pvec = np.concatenate([
        _fm(inp["attn_norm"]), _fm(inp["ffn_norm"]), _fm(inp["mix_norm"]), _fm(inp["final_norm"]),
        _fm(inp["conv_w"]), _fm(inp["lru_conv_w"]), _fm(inp["lru_conv_b"]), _fm(inp["lru_ba"]),
        _fm(inp["lru_bx"]), _fm(inp["lru_lambda"])], axis=1)
    assert pvec.shape == (128, PV_COLS)
    bd = np.zeros((2, 128, 8 * 128), f32)
    for L in range(2):
        for gi, nm in enumerate(("lru_wa", "lru_wx")):
            w = np.asarray(inp[nm], f32)[L]
            for j in range(4):
                for hh in range(2):
                    c0 = (gi * 4 + j) * 128 + hh * 64
                    bd[L, hh * 64:(hh + 1) * 64, c0:c0 + 64] = w[2 * j + hh]
    sinks = np.asarray(inp["attn_sinks"], f32)
    srow = np.zeros((2, 4, 4, 128), f32)
    for L in range(2):
        for kv in range(2):
            for par in range(2):
                for i in range(4):
                    srow[L, kv * 2 + par, i, :] = sinks[L, kv * 8 + 2 * i + par]
    srow = np.ascontiguousarray(np.broadcast_to(srow.reshape(1, -1), (128, 4096)))
    import ml_dtypes
    kk = np.arange(128)[:, None]
    qq = np.arange(128)[None, :]
    m_prev = np.tile((kk > qq).astype(f32), (1, 4))
    m_cur = np.tile((kk <= qq).astype(f32), (1, 4))
    ident = np.eye(128, dtype=f32)
    sel = np.zeros((8, NE * 128), f32)
    for e in range(NE):
        sel[e, e * 128:(e + 1) * 128] = 1.0
    shared = {
        "w_in_r": w_in_r,
        "w_out": np.ascontiguousarray(np.asarray(inp["w_out"], f32)),
        "dense_wg": np.ascontiguousarray(np.asarray(inp["dense_w_gate"], f32)[0]),
        "dense_wu": np.ascontiguousarray(np.asarray(inp["dense_w_up"], f32)[0]),
        "dense_wd": np.ascontiguousarray(np.asarray(inp["dense_w_down"], f32)[0]),
        "exp_wg": np.ascontiguousarray(np.asarray(inp["expert_w_gate"], f32)[0]),
        "exp_wu": np.ascontiguousarray(np.asarray(inp["expert_w_up"], f32)[0]),
        "exp_wd": np.ascontiguousarray(np.asarray(inp["expert_w_down"], f32)[0]),
        "router": np.ascontiguousarray(np.asarray(inp["router_w"], f32)[0].reshape(KC, 128, NE).transpose(1, 0, 2)),
        "pvec": pvec,
        "lru_bd": bd,
        "sinks_row": srow,
        "ident": ident,
        "selmat": sel,
    }
    return shared, m_prev, m_cur


def _x_tiles(xseq, NT):
    return np.ascontiguousarray(xseq.reshape(NT, T, KC, 128).transpose(0, 3, 2, 1))


def _untile(o, NT):
    return np.ascontiguousarray(o.transpose(0, 3, 2, 1).reshape(NT * T, D))


_CACHE = {}


def run(inp, n_seq, NH, n_layers=2, dbg=None, final=True, moe=True, trace=False):
    import ml_dtypes
    shared, m_prev, m_cur = _prep_shared(inp)
    x = np.asarray(inp["x"], np.float32)
    zeros = np.zeros_like(m_prev)
    in_maps = []
    for c in range(n_seq):
        tiles = _x_tiles(x[c, :2 * NH * T], 2 * NH)
        for odd in range(2):
            m = dict(shared)
            if odd:
                m["xT"] = tiles
            else:
                m["xT"] = np.ascontiguousarray(np.concatenate([tiles[:NH], tiles[:NH]], axis=0))
            m["masks"] = np.concatenate([m_prev, m_cur, zeros, m_prev if odd else zeros], axis=1).astype(ml_dtypes.bfloat16)
            m["flag"] = np.full((128, 1), 1.0 if odd else 0.0, np.float32)
            in_maps.append(m)
    key = (NH, n_layers, str(dbg), final, moe)
    if key not in _CACHE:
        _CACHE[key] = Prog(NH, n_layers, dbg=dbg, final=final, moe=moe).build()
    nc = _CACHE[key]
    res = run_bass_kernel_spmd(nc, in_maps, core_ids=list(range(2 * n_seq)), trace=trace)
    outs = []
    for c in range(n_seq):
        outs.append(np.concatenate([_untile(np.asarray(res.results[2 * c + odd]["outT"]), NH) for odd in range(2)], axis=0))
    return outs, res


def kernel(**inputs):
    outs, _ = run(inputs, 4, 4)
    return np.stack(outs, axis=0).astype(np.float32)
```
